# Optimizing a Trainium2 kernel written in Bass

```python
import math
import jax, jax.numpy as jnp
from jax import lax
import numpy as np

D_MODEL = 1024
BATCH = 8
SEQ = 2048
DEPTH = 1

MEM_LEN = 256
MEM_HEADS = 4
MEM_HEAD_DIM = D_MODEL // MEM_HEADS
A_HEADS = 8
A_HEAD_DIM = 64
A_WIDTH = A_HEADS * A_HEAD_DIM
MOBA_BLOCK = 256
MOBA_TOPK = 3
B_HEADS = 4
B_QK_DIM = 64
B_V_DIM = 2 * B_QK_DIM
B_QK_WIDTH = B_HEADS * 2 * B_QK_DIM
B_WIDTH = B_HEADS * B_V_DIM
MIX_WIDTH = A_WIDTH + B_WIDTH
IN_WIDTH = 3 * A_WIDTH + 2 * B_QK_WIDTH + B_WIDTH
IN_SPLITS = [A_WIDTH, 2 * A_WIDTH, 3 * A_WIDTH, 3 * A_WIDTH + B_QK_WIDTH, 3 * A_WIDTH + 2 * B_QK_WIDTH]
N_ATT_HEADS = A_HEADS + B_HEADS
REL_BUCKETS = 32
REL_MAX_DIST = 128
D_FF = -(-8 * D_MODEL // (3 * 256)) * 256
Q_BLOCK = 128
EPS = 1e-6
NEG_INF = -1e30

kernel_name = "hybrid_moba_diffattn_parallel_heads"


def rms_norm(x, g):
    xf = x.astype(jnp.float32)
    y = xf * lax.rsqrt(jnp.mean(xf * xf, axis=-1, keepdims=True) + EPS)
    return (y * g.astype(jnp.float32)).astype(x.dtype)


def rel_bucket(dist):
    n = jnp.maximum(dist, 0)
    max_exact = REL_BUCKETS // 2
    log_ratio = jnp.log(jnp.maximum(n, max_exact).astype(jnp.float32) / max_exact) / math.log(REL_MAX_DIST / max_exact)
    large = max_exact + (log_ratio * (REL_BUCKETS - max_exact)).astype(jnp.int32)
    large = jnp.minimum(large, REL_BUCKETS - 1)
    return jnp.where(n < max_exact, n, large)


def moba_attention(q, k, v, table_a):
    B_, S, H, dh = q.shape
    nb = -(-S // MOBA_BLOCK)
    pad = nb * MOBA_BLOCK - S
    topk = min(MOBA_TOPK, nb)
    n_chunks = S // Q_BLOCK
    scale = dh ** -0.5
    bias_ht = table_a.T.astype(jnp.float32)
    h_i = jnp.arange(H)[:, None, None]
    blk_ids = jnp.arange(nb)
    key_off = jnp.arange(MOBA_BLOCK)

    def per_batch(args):
        q1, k1, v1 = args
        kp = jnp.pad(k1, ((0, pad), (0, 0), (0, 0))).reshape(nb, MOBA_BLOCK, H, dh).transpose(2, 0, 1, 3)
        vp = jnp.pad(v1, ((0, pad), (0, 0), (0, 0))).reshape(nb, MOBA_BLOCK, H, dh).transpose(2, 0, 1, 3)
        kmean = jnp.mean(kp.astype(jnp.float32), axis=2)
        qc = q1.reshape(n_chunks, Q_BLOCK, H, dh).transpose(0, 2, 1, 3)

        def per_chunk(args2):
            qblk, c = args2
            t = c * Q_BLOCK + jnp.arange(Q_BLOCK)
            own = t // MOBA_BLOCK
            gate = jnp.einsum('hqd,hnd->hqn', qblk.astype(jnp.float32), kmean)
            gate = jnp.where(blk_ids[None, None, :] < own[None, :, None], gate, NEG_INF)
            _, sel = lax.top_k(gate, topk)
            idx = jnp.concatenate([sel, jnp.broadcast_to(own[None, :, None], (H, Q_BLOCK, 1))], axis=-1)
            k_sel = kp[h_i, idx]
            v_sel = vp[h_i, idx]
            logits = jnp.einsum('hqd,hqtld->hqtl', qblk, k_sel, preferred_element_type=jnp.float32) * scale
            kpos = idx[..., None] * MOBA_BLOCK + key_off
            dist = t[None, :, None, None] - kpos
            logits = logits + bias_ht[h_i[..., None], rel_bucket(dist)]
            slot_ok = jnp.concatenate([jnp.arange(topk)[None, :] < own[:, None],
                                       jnp.ones((Q_BLOCK, 1), dtype=bool)], axis=1)
            mask = slot_ok[None, :, :, None] & (dist >= 0)
            logits = jnp.where(mask, logits, NEG_INF)
            p = jax.nn.softmax(logits.reshape(H, Q_BLOCK, -1), axis=-1).reshape(logits.shape)
            out = jnp.einsum('hqtl,hqtld->hqd', p.astype(v_sel.dtype), v_sel, preferred_element_type=jnp.float32)
            return out.astype(q.dtype)

        outs = lax.map(per_chunk, (qc, jnp.arange(n_chunks)))
        return outs.transpose(0, 2, 1, 3).reshape(S, H, dh)

    return lax.map(per_batch, (q, k, v))


def diff_attention(q, k, v, table_b, lam, subln_g, lambda_init):
    B_, S, H, _, dq = q.shape
    n_chunks = S // Q_BLOCK
    scale = dq ** -0.5
    kt = k.transpose(0, 2, 3, 1, 4)
    vt = v.transpose(0, 2, 1, 3)
    qc = q.reshape(B_, n_chunks, Q_BLOCK, H, 2, dq).transpose(1, 0, 3, 4, 2, 5)
    bias_ht = table_b.T.astype(jnp.float32)
    s_pos = jnp.arange(S)

    def per_chunk(args):
        qblk, c = args
        t = c * Q_BLOCK + jnp.arange(Q_BLOCK)
        dist = t[:, None] - s_pos[None, :]
        bias = bias_ht[:, rel_bucket(dist)]
        logits = jnp.einsum('bhmqd,bhmsd->bhmqs', qblk, kt, preferred_element_type=jnp.float32) * scale
        logits = jnp.where(dist >= 0, logits + bias[None, :, None], NEG_INF)
        p = jax.nn.softmax(logits, axis=-1)
        attn = p[:, :, 0] - lam * p[:, :, 1]
        return jnp.einsum('bhqs,bhsd->bhqd', attn.astype(v.dtype), vt, preferred_element_type=jnp.float32)

    outs = lax.map(per_chunk, (qc, jnp.arange(n_chunks)))
    o = outs.transpose(1, 0, 3, 2, 4).reshape(B_, S, H, -1)
    o = rms_norm(o, subln_g) * (1.0 - lambda_init)
    return o.astype(v.dtype)


def memory_cross_attention(x, mem, norm_g, mem_g, wq, wk, wv, wo):
    B_, S, _ = x.shape
    M = mem.shape[1]
    h = rms_norm(x, norm_g)
    m = rms_norm(mem, mem_g)
    q = (h @ wq).reshape(B_, S, MEM_HEADS, MEM_HEAD_DIM)
    k = (m @ wk).reshape(B_, M, MEM_HEADS, MEM_HEAD_DIM)
    v = (m @ wv).reshape(B_, M, MEM_HEADS, MEM_HEAD_DIM)
    logits = jnp.einsum('bshd,bmhd->bhsm', q, k, preferred_element_type=jnp.float32) * (MEM_HEAD_DIM ** -0.5)
    p = jax.nn.softmax(logits, axis=-1)
    o = jnp.einsum('bhsm,bmhd->bshd', p.astype(v.dtype), v, preferred_element_type=jnp.float32).astype(x.dtype)
    return o.reshape(B_, S, MEM_HEADS * MEM_HEAD_DIM) @ wo


def swiglu(x, norm_g, w_gate, w_up, w_down):
    h = rms_norm(x, norm_g)
    return (jax.nn.silu(h @ w_gate) * (h @ w_up)) @ w_down


def setup_inputs(seed: int = 0) -> dict:
    key = jax.random.key(seed)
    ks = jax.random.split(key, 24)
    f32 = jnp.float32

    def w(k, shape, fan_in):
        return jax.random.normal(k, shape, f32) * (fan_in ** -0.5)

    def gain(k, shape):
        return 1.0 + 0.02 * jax.random.normal(k, shape, f32)

    return {
        "x": jax.random.normal(ks[0], (BATCH, SEQ, D_MODEL), f32),
        "mem": jax.random.normal(ks[1], (BATCH, MEM_LEN, D_MODEL), f32),
        "mix_norm_g": gain(ks[2], (DEPTH, D_MODEL)),
        "w_in": w(ks[3], (DEPTH, D_MODEL, IN_WIDTH), D_MODEL),
        "moba_out_g": gain(ks[4], (DEPTH, A_WIDTH)),
        "diff_lambda": 0.1 * jax.random.normal(ks[5], (DEPTH, 4, B_QK_DIM), f32),
        "diff_subln_g": gain(ks[6], (DEPTH, B_V_DIM)),
        "w_out": w(ks[7], (DEPTH, MIX_WIDTH, D_MODEL), MIX_WIDTH),
        "rel_bias_table": 0.5 * jax.random.normal(ks[8], (REL_BUCKETS, N_ATT_HEADS), f32),
        "cross_norm_g": gain(ks[9], (DEPTH, D_MODEL)),
        "mem_norm_g": gain(ks[10], (DEPTH, D_MODEL)),
        "w_cq": w(ks[11], (DEPTH, D_MODEL, MEM_HEADS * MEM_HEAD_DIM), D_MODEL),
        "w_ck": w(ks[12], (DEPTH, D_MODEL, MEM_HEADS * MEM_HEAD_DIM), D_MODEL),
        "w_cv": w(ks[13], (DEPTH, D_MODEL, MEM_HEADS * MEM_HEAD_DIM), D_MODEL),
        "w_co": w(ks[14], (DEPTH, MEM_HEADS * MEM_HEAD_DIM, D_MODEL), MEM_HEADS * MEM_HEAD_DIM),
        "ffn_norm_g": gain(ks[15], (DEPTH, D_MODEL)),
        "w_gate": w(ks[16], (DEPTH, D_MODEL, D_FF), D_MODEL),
        "w_up": w(ks[17], (DEPTH, D_MODEL, D_FF), D_MODEL),
        "w_down": w(ks[18], (DEPTH, D_FF, D_MODEL), D_FF),
        "final_norm_g": gain(ks[19], (D_MODEL,)),
    }


def reference(x, mem, mix_norm_g, w_in, moba_out_g, diff_lambda, diff_subln_g, w_out, rel_bias_table,
              cross_norm_g, mem_norm_g, w_cq, w_ck, w_cv, w_co, ffn_norm_g, w_gate, w_up, w_down,
              final_norm_g):
    B_, S, _ = x.shape
    table_a = rel_bias_table[:, :A_HEADS]
    table_b = rel_bias_table[:, A_HEADS:]
    for l in range(DEPTH):
        h = rms_norm(x, mix_norm_g[l])
        proj = jnp.einsum('bsd,de->bse', h, w_in[l])
        qa, ka, va, qb, kb, vb = jnp.split(proj, IN_SPLITS, axis=-1)
        oa = moba_attention(qa.reshape(B_, S, A_HEADS, A_HEAD_DIM),
                            ka.reshape(B_, S, A_HEADS, A_HEAD_DIM),
                            va.reshape(B_, S, A_HEADS, A_HEAD_DIM), table_a)
        oa = rms_norm(oa.reshape(B_, S, A_WIDTH), moba_out_g[l])
        lambda_init = 0.8 - 0.6 * math.exp(-0.3 * l)
        lp = diff_lambda[l].astype(jnp.float32)
        lam = jnp.exp(jnp.sum(lp[0] * lp[1])) - jnp.exp(jnp.sum(lp[2] * lp[3])) + lambda_init
        ob = diff_attention(qb.reshape(B_, S, B_HEADS, 2, B_QK_DIM),
                            kb.reshape(B_, S, B_HEADS, 2, B_QK_DIM),
                            vb.reshape(B_, S, B_HEADS, B_V_DIM),
                            table_b, lam, diff_subln_g[l], lambda_init)
        ob = ob.reshape(B_, S, B_WIDTH)
        mixed = jnp.concatenate([oa, ob], axis=-1)
        x = x + jnp.einsum('bse,ed->bsd', mixed, w_out[l])
        x = x + memory_cross_attention(x, mem, cross_norm_g[l], mem_norm_g[l], w_cq[l], w_ck[l], w_cv[l], w_co[l])
        x = x + swiglu(x, ffn_norm_g[l], w_gate[l], w_up[l], w_down[l])
    return rms_norm(x, final_norm_g)
```

```python
import math
import numpy as np
import concourse.bass as bass
import concourse.mybir as mybir
from concourse.bass_utils import run_bass_kernel_spmd

F32 = mybir.dt.float32
BF16 = mybir.dt.bfloat16
AF = mybir.ActivationFunctionType
ALU = mybir.AluOpType
AX = mybir.AxisListType

S = 2048
D = 1024
NT = 16
DC = 8
ML = 256
DFF = 2816
EPS = 1e-6
NEGM = 30000.0

DEBUG_OUT = None
STOP_AFTER = None


class _Op:
    __slots__ = ("eng", "fn", "deps", "needed", "dma_sem", "val", "tag")


class Sched:
    ENGS = ("pe", "act", "dve", "pool", "sp")

    def __init__(self):
        self.q = {e: [] for e in self.ENGS}
        self.lw = {}
        self.rd = {}

    def add(self, eng, fn, reads=(), writes=(), dma_sem=None, tag=""):
        op = _Op()
        op.eng = eng
        op.fn = fn
        op.dma_sem = dma_sem
        op.needed = False
        op.val = 0
        op.tag = tag
        deps = []
        seen = set()

        def _add(d):
            if d is None or id(d) in seen:
                return
            seen.add(id(d))
            if d.dma_sem is None and d.eng == "pe" and eng == "pe" and dma_sem is None:
                return
            deps.append(d)

        for r in reads:
            _add(self.lw.get(r))
        for w in writes:
            _add(self.lw.get(w))
            for r in self.rd.get(w, ()):
                _add(r)
        op.deps = deps
        for d in deps:
            d.needed = True
        for r in reads:
            lst = self.rd.setdefault(r, [])
            if dma_sem is None:
                lst[:] = [o for o in lst if not (o.dma_sem is None and o.eng == eng)]
            lst.append(op)
        for w in writes:
            self.lw[w] = op
            self.rd[w] = []
        self.q[eng].append(op)
        return op

    def finalize(self):
        cnt = {}
        for e in self.ENGS:
            for op in self.q[e]:
                if op.dma_sem is not None:
                    cnt[op.dma_sem] = cnt.get(op.dma_sem, 0) + 16
                    op.val = cnt[op.dma_sem]
                elif op.needed:
                    cnt[e] = cnt.get(e, 0) + 1
                    op.val = cnt[e]

    def emit(self, eng, h, semh):
        known = {}
        for op in self.q[eng]:
            waits = {}
            for d in op.deps:
                key = d.dma_sem if d.dma_sem is not None else d.eng
                if d.val > waits.get(key, 0):
                    waits[key] = d.val
            for key, val in waits.items():
                if known.get(key, 0) >= val:
                    continue
                h.wait_ge(semh[key], val)
                known[key] = val
            ins = op.fn(h)
            if op.dma_sem is not None:
                ins.then_inc(semh[op.dma_sem], 16)
            elif op.needed:
                ins.then_inc(semh[eng], 1)


def _rel_bucket_np(n):
    n = np.maximum(n, 0)
    max_exact = 16
    nf = np.maximum(n, max_exact).astype(np.float32)
    log_ratio = np.log(nf / np.float32(max_exact)) / np.float32(math.log(128 / max_exact))
    large = max_exact + (log_ratio * np.float32(32 - max_exact)).astype(np.int32)
    large = np.minimum(large, 31)
    return np.where(n < max_exact, n, large)


def _host_consts():
    c = {}
    c["c_ident"] = np.eye(128, dtype=np.float32)
    urp = np.zeros((33, 384), np.float32)
    for m in range(383):
        n = m - 127
        if n >= 0:
            b = int(_rel_bucket_np(np.array([n]))[0])
            urp[b, m] += 1.0
            urp[31, m] -= 1.0
        else:
            urp[32, m] = 1.0
    c["c_jmat"] = np.ascontiguousarray(np.eye(128, dtype=np.float32)[::-1])
    c["c_urp"] = urp
    koh = np.zeros((8, S), np.float32)
    for j in range(8):
        koh[j, j * 256:(j + 1) * 256] = 1.0
    c["c_koh"] = koh
    negmask = np.zeros((128, 16, 8), np.float32)
    ownm1 = np.full((128, 16, 8), -1.0, np.float32)
    for qt in range(16):
        own = qt // 2
        negmask[:, qt, own:] = -1e30
        ownm1[:, qt, own] = 0.0
    c["c_negmask"] = negmask.reshape(128, 128)
    c["c_ownm1"] = ownm1.reshape(128, 128)
    return c


def build_program():
    nc = bass.Bass("TRN2", target_bir_lowering=False)

    def din(name, shape):
        return nc.dram_tensor(name, list(shape), F32, kind="ExternalInput").ap()

    x_d = din("x", [S, D])
    mem_d = din("mem", [ML, D])
    g_mix_d = din("mix_norm_g", [D])
    w_in_d = din("w_in", [D, 3072])
    moba_g_d = din("moba_out_g", [128, 4])
    lam_d = din("diff_lambda", [256])
    subln_d = din("diff_subln_g", [128, 1])
    w_out_d = din("w_out", [D, D])
    tab_d = din("rel_bias_table", [32, 12])
    g_cross_d = din("cross_norm_g", [D])
    g_mem_d = din("mem_norm_g", [D])
    w_cq_d = din("w_cq", [D, D])
    w_ck_d = din("w_ck", [D, D])
    w_cv_d = din("w_cv", [D, D])
    w_co_d = din("w_co", [D, D])
    g_ffn_d = din("ffn_norm_g", [D])
    w_gate_d = din("w_gate", [D, DFF])
    w_up_d = din("w_up", [D, DFF])
    w_down_d = din("w_down", [DFF, D])
    g_fin_d = din("final_norm_g", [D])
    c_ident_d = din("c_ident", [128, 128])
    c_urp_d = din("c_urp", [33, 384])
    c_koh_d = din("c_koh", [8, S])
    c_negmask_d = din("c_negmask", [128, 128])
    c_ownm1_d = din("c_ownm1", [128, 128])
    c_jmat_d = din("c_jmat", [128, 128])
    bv_d = nc.dram_tensor("bv_scr", [12, 384], F32).ap()
    out_d = nc.dram_tensor("out", [S, D], F32, kind="ExternalOutput").ap()

    dbg_d = {}
    if DEBUG_OUT:
        for name, shape in DEBUG_OUT.items():
            dbg_d[name] = nc.dram_tensor("dbg_" + name, list(shape), F32, kind="ExternalOutput").ap()

    sch = Sched()
    sem_names = ["pe", "act", "dve", "pool"]
    sem_names += ["x%d" % t for t in range(NT)]
    sem_names += ["ws%d" % i for i in range(4)] + ["wsub%d" % i for i in range(4)]
    sem_names += ["gb0", "gb1", "xtmp0", "xtmp1", "xre0", "xre1", "outs", "dbg"]
    sem_names += ["k%d" % i for i in range(16)]

    from contextlib import ExitStack
    with ExitStack() as es:
        def sb(name, shape, dt):
            return es.enter_context(nc.sbuf_tensor(name, list(shape), dt))

        arena = sb("arena", [128, 32768], BF16)
        hT_t = sb("hT", [128, DC * S], BF16)
        a2 = sb("arena2", [128, 16384], BF16)
        ws_t = sb("ws", [128, 4 * 4096], BF16)
        TT_t = sb("TT", [128, 12 * 2 * 128], BF16)
        gB_t = sb("gB", [128, 2 * D], F32)
        xtmp_t = sb("xtmp", [128, 2 * D], F32)
        identb = sb("identb", [128, 128], BF16)
        I3 = sb("I3", [128, 128], BF16)
        ones_b = sb("ones_b", [128, 128], BF16)
        Jb = sb("Jb", [128, 128], BF16)
        bvs = sb("bvs", [12, 384], F32)
        urp = sb("urp", [33, 384], F32)
        tabx = sb("tabx", [33, 12], F32)
        negmask = sb("negmask", [128, 128], F32)
        ownm1 = sb("ownm1", [128, 128], F32)
        g2 = sb("g2", [128, 128], F32)
        top8 = sb("top8", [128, 128], F32)
        selt = sb("selt", [128, 128], F32)
        mpad = sb("mpad", [128, 16 * 72], BF16)
        kmf = sb("kmf", [128, 16], F32)
        kmb = sb("kmb", [128, 2 * 16], BF16)
        ss = sb("ss", [128, 18], F32)
        sd = sb("sd", [128, 18], F32)
        rstd = sb("rstd", [128, 18], F32)
        epsb = sb("epsb", [128, 1], F32)
        rcb_t = sb("rcb", [128, 4 * 512], F32)
        gA = sb("gA", [128, 4], F32)
        gS = sb("gS", [128, 1], F32)
        gS8 = sb("gS8", [128, 1], F32)
        mTp_t = sb("mTp", [128, 2048], BF16)
        lamt = sb("lamt", [128, 256], F32)
        lamw = sb("lamw", [128, 128], F32)
        lams = sb("lams", [128, 8], F32)

        ps = [es.enter_context(nc.psum_tensor("ps%d" % i, [128, 512], F32)) for i in range(8)]
        semh = {n: es.enter_context(nc.semaphore(n)) for n in sem_names}
        block = es.enter_context(nc.Block())

        xs = arena[:, :].bitcast(F32).rearrange("p (t d) -> p t d", t=NT)
        mixT = arena[:, 0:16384].rearrange("p (t c k) -> p t c k", t=NT, c=8)
        Va = arena[:, 16384:32768].rearrange("p (t h k) -> p t h k", t=NT, h=8)
        Vb = arena[:, 16384:24576].rearrange("p (t h k) -> p t h k", t=NT, h=4)
        hT = hT_t[:, :].rearrange("p (c s) -> p c s", c=DC)
        QK = [a2[:, i * 2048:(i + 1) * 2048] for i in range(6)]
        NPT = 7
        PT = [a2[:, 12288 + i * 512: 12288 + (i + 1) * 512] for i in range(4)] + \
             [a2[:, 14848 + i * 512: 14848 + (i + 1) * 512] for i in range(3)]
        osq = a2[:, 14336:14848]
        hbf = [a2[:, 12288 + i * 1024: 12288 + (i + 1) * 1024] for i in range(2)]
        ocT = [a2[:, i * 4096:(i + 1) * 4096].rearrange("p (c k) -> p c k", c=8) for i in range(2)]
        QcT = [a2[:, 8192 + i * 1024: 8192 + (i + 1) * 1024].rearrange("p (c k) -> p c k", c=2) for i in range(2)]
        KcT = a2[:, 10240:12288].rearrange("p (c k) -> p c k", c=8)
        Vc = a2[:, 14336:16384].rearrange("p (m k) -> p m k", m=2)
        mT = a2[:, 4096:6144].rearrange("p (c k) -> p c k", c=8)
        actT = [a2[:, i * 8192:(i + 1) * 8192].rearrange("p (c k) -> p c k", c=4) for i in range(2)]
        wsl = [ws_t[:, i * 4096:(i + 1) * 4096] for i in range(4)]
        TT = TT_t[:, :].rearrange("p (h t q) -> p h t q", h=12, t=2)
        gB = [gB_t[:, i * D:(i + 1) * D] for i in range(2)]
        xtmp = [xtmp_t[:, i * D:(i + 1) * D] for i in range(2)]
        rcb = [rcb_t[:, i * 512:(i + 1) * 512] for i in range(4)]
        junk = rcb_t[:, 1024:1536].bitcast(BF16)
        mpad3 = mpad[:, :].rearrange("p (t k) -> p t k", t=16)
        mTp = mTp_t[:, :].rearrange("p (c k) -> p c k", c=8)
        KV = xtmp_t[:, :].bitcast(BF16)
        KcT2 = KV[:, 0:2048].rearrange("p (c k) -> p c k", c=8)
        Vc2 = KV[:, 2048:4096].rearrange("p (m k) -> p m k", m=2)
        xre = [a2[:, 0:2048].bitcast(F32), a2[:, 2048:4096].bitcast(F32)]
        kmb4 = kmb[:, :].rearrange("p (b l j) -> p b l j", b=2, l=2)

        def r_xs(t):
            return ["ar%d" % (2 * t), "ar%d" % (2 * t + 1)]

        def r_mixT(t):
            return ["ar%d" % t]

        def r_Va(t):
            return ["ar%d" % (16 + t)]

        def r_Vb(t):
            return ["ar%d" % (16 + t // 2)]

        def r_a2(lo, hi):
            return ["a2_%d" % g for g in range(lo // 512, (hi + 511) // 512)]

        def r_QK(i, qc=None):
            if qc is None:
                return r_a2(i * 2048, (i + 1) * 2048)
            return r_a2(i * 2048 + qc * 512, i * 2048 + (qc + 1) * 512)

        def r_PT(i):
            if i >= 4:
                return r_a2(14848 + (i - 4) * 512, 14848 + (i - 3) * 512)
            return r_a2(12288 + i * 512, 12288 + (i + 1) * 512)

        def r_hbf(i):
            return r_a2(12288 + i * 1024, 12288 + (i + 1) * 1024)

        R_OSQ = r_a2(14336, 14848)
        R_JUNK = ["rcb2"]

        def r_ws(i):
            if i == 2:
                return ["wsub%d" % k for k in range(4)]
            return ["ws%d" % i]

        def r_hT(lo_tile, hi_tile):
            return ["hT%d" % t for t in range(lo_tile, hi_tile)]

        def r_ps(b):
            return ["ps%d" % b]

        stop = {"flag": False}

        def dma(queue, out, in_, sem, reads=(), writes=(), tag=""):
            def fn(h, out=out, in_=in_):
                return h.dma_start(out=out, in_=in_)
            return sch.add(queue, fn, reads=reads, writes=writes, dma_sem=sem, tag=tag)

        def mm_group(out, pairs, reads, writes, start_first=True, stop_last=True, tag=""):
            def fn(h, out=out, pairs=pairs):
                ins = None
                n = len(pairs)
                for i, (l, r) in enumerate(pairs):
                    ins = h.matmul(out, l, r, start=(start_first and i == 0), stop=(stop_last and i == n - 1))
                return ins
            return sch.add("pe", fn, reads=reads, writes=writes, tag=tag)

        def act(out, in_, func, reads, writes, scale=1.0, bias=None, accum_out=None, tag=""):
            def fn(h, out=out, in_=in_):
                kw = {}
                if bias is not None:
                    kw["bias"] = bias
                if accum_out is not None:
                    kw["accum_out"] = accum_out
                return h.activation(out=out, in_=in_, func=func, scale=scale, **kw)
            return sch.add("act", fn, reads=reads, writes=writes, tag=tag)

        def vop(eng, fn, reads, writes, tag=""):
            return sch.add(eng, fn, reads=reads, writes=writes, tag=tag)

        evac_rr = {"i": 0}

        def evac(out, in_, reads, writes, scale=None, eng=None, tag=""):
            if eng is None:
                eng = ("act", "dve")[evac_rr["i"] % 2]
                evac_rr["i"] += 1
            if eng == "act":
                return act(out, in_, AF.Copy, reads, writes, scale=(1.0 if scale is None else scale), tag=tag)
            if scale is None:
                return vop("dve", lambda h, out=out, in_=in_: h.tensor_copy(out=out, in_=in_), reads, writes, tag=tag)
            return vop("dve", lambda h, out=out, in_=in_: h.tensor_scalar(
                out=out, in0=in_, scalar1=float(scale), scalar2=None, op0=ALU.mult), reads, writes, tag=tag)

        def dbg_dump(name, src_ap, reads, rows=128):
            if name not in dbg_d:
                return
            dst = dbg_d[name]
            dma("pool", dst, src_ap, "dbg", reads=reads, writes=["dbgout_" + name])

        def load_panel(slot, w_d, col0, ncols, row0=0, nchunks=8, tag=""):
            src = w_d[row0:row0 + nchunks * 128, col0:col0 + ncols].rearrange("(c p) n -> p c n", p=128)
            dst = wsl[slot][:, 0:nchunks * ncols].rearrange("p (c n) -> p c n", c=nchunks)
            dma("pool", dst, src, "ws%d" % slot, writes=r_ws(slot), tag=tag)
            return dst

        def load_sub(sub, w_d, col0, tag=""):
            src = w_d[:, col0:col0 + 128].rearrange("(c p) n -> p c n", p=128)
            dst = wsl[2][:, sub * 1024:(sub + 1) * 1024].rearrange("p (c n) -> p c n", c=8)
            dma("pool", dst, src, "wsub%d" % sub, writes=["wsub%d" % sub], tag=tag)
            return dst

        dma("sp", gB[0], g_mix_d.partition_broadcast(128), "gb0", writes=["gb0"])
        dma("sp", gB[1], g_mem_d.partition_broadcast(128), "gb1", writes=["gb1"])
        dma("sp", urp[:, :], c_urp_d[:, :], "k0", writes=["urp"])
        dma("sp", tabx[0:32, :], tab_d[:, :], "k1", writes=["tabx"])
        dma("sp", negmask[:, :], c_negmask_d[:, :], "k2", writes=["negmask"])
        dma("sp", ownm1[:, :], c_ownm1_d[:, :], "k3", writes=["ownm1"])
        dma("sp", gA[:, :], moba_g_d[:, :], "k4", writes=["gA"])
        dma("sp", gS[:, :], subln_d[:, :], "k5", writes=["gS"])
        dma("sp", lamt[:, :], lam_d.partition_broadcast(128), "k6", writes=["lamt"])
        for t in list(range(8, 16)) + list(range(0, 8)):
            dma("sp", xs[:, t, :], x_d[t * 128:(t + 1) * 128, :], "x%d" % t, writes=r_xs(t), tag="xload")
        for mt in range(2):
            dma("sp", xtmp[mt], mem_d[mt * 128:(mt + 1) * 128, :], "xtmp%d" % mt, writes=["xtmp%d" % mt], tag="memld")
        dma("pool", identb[:, :], c_ident_d[:, :], "k7", writes=["identb"])
        dma("pool", Jb[:, :], c_jmat_d[:, :], "k11", writes=["Jb"])
        dma("pool", QK[2][64:72, :], c_koh_d[:, :], "k8", writes=r_QK(2))
        dma("pool", QK[3][64:72, :], c_koh_d[:, :], "k9", writes=r_QK(3))
        dma("pool", QK[5][64:72, :], c_koh_d[:, :], "k10", writes=r_QK(5))
        wVa = load_panel(0, w_in_d, 1024, 512, tag="wVa")
        wVb = load_panel(1, w_in_d, 2560, 512, tag="wVb")
        kvw = {"p": load_panel(3, w_ck_d, 0, 512, tag="wck0")}
        pre_sub = (load_sub(0, w_in_d, 0, tag="wqa"), load_sub(1, w_in_d, 512, tag="wka"))

        vop("dve", lambda h: h.memset(epsb[:, :], EPS), [], ["epsb"])
        vop("dve", lambda h: h.memset(ss[:, :], 0.0), [], ["ss%d" % c for c in range(18)])
        vop("dve", lambda h: h.memset(ones_b[:, :], 1.0), [], ["ones_b"])
        vop("dve", lambda h: h.memset(mpad[:, :], 0.0), [], ["mpad"])
        vop("dve", lambda h: h.memset(kmb[:, :], 0.0), [], ["kmb"])
        vop("dve", lambda h: h.memset(tabx[32:33, :], -NEGM), ["tabx"], ["tabx"])
        vop("pool", lambda h: h.memset(QK[0][64:72, :], 0.0), [], r_QK(0))
        vop("pool", lambda h: h.memset(QK[1][64:72, :], 0.0), [], r_QK(1))
        vop("dve", lambda h: h.tensor_scalar(out=I3[:, :], in0=identb[:, :], scalar1=NEGM, scalar2=None, op0=ALU.mult),
            ["identb"], ["I3"])
        vop("dve", lambda h: h.tensor_scalar(out=gS8[:, :], in0=gS[:, :], scalar1=0.8, scalar2=None, op0=ALU.mult),
            ["gS"], ["gS8"])

        vop("dve", lambda h: h.tensor_tensor(out=lamw[:, 0:64], in0=lamt[:, 0:64], in1=lamt[:, 64:128], op=ALU.mult),
            ["lamt"], ["lamw"])
        vop("dve", lambda h: h.tensor_tensor(out=lamw[:, 64:128], in0=lamt[:, 128:192], in1=lamt[:, 192:256], op=ALU.mult),
            ["lamt", "lamw"], ["lamw"])
        vop("dve", lambda h: h.tensor_reduce(out=lams[:, 0:2], in_=lamw[:, :].rearrange("p (a b) -> p a b", a=2),
                                              axis=AX.X, op=ALU.add), ["lamw"], ["lams"])
        act(lams[:, 2:4], lams[:, 0:2], AF.Exp, ["lams"], ["lams"])
        vop("dve", lambda h: h.scalar_tensor_tensor(out=lams[:, 4:5], in0=lams[:, 3:4], scalar=-0.2, in1=lams[:, 2:3],
                                                     op0=ALU.add, op1=ALU.subtract), ["lams"], ["lams"])
        neglam = lams[:, 4:5]

        sch.add("pe", lambda h: h.matmul(ps[0][0:12, 0:384], tabx[0:33, 0:12], urp[0:33, 0:384], start=True, stop=True),
                reads=["urp", "tabx"], writes=r_ps(0), tag="bv_mm")
        evac(bvs[:, :], ps[0][0:12, 0:384], r_ps(0), ["bvs"], eng="dve", tag="bv_ev")
        dma("sp", bv_d[:, :], bvs[:, :], "k12", reads=["bvs"], writes=["bv_dram"])
        for tsel in range(2):
            src = bass.AP(tensor=bv_d.tensor, offset=tsel * 128, ap=[[1, 128], [384, 12], [1, 128]])
            dma("pool", TT[:, :, tsel, :], src, "k%d" % (13 + tsel), reads=["bv_dram"], writes=["TT%d" % tsel])

        class NormStream:
            def __init__(self, src_tile, src_reads, gslot, dst, dst_regions, stat_off, tagp, banks=(6, 7)):
                self.src_tile, self.src_reads, self.gslot = src_tile, src_reads, gslot
                self.dst, self.dst_regions, self.stat_off, self.tagp, self.banks = dst, dst_regions, stat_off, tagp, banks
                self.tiles = []

            def _s1(self, k):
                t = self.tiles[k]
                col = self.stat_off + t
                act(junk, self.src_tile(t), AF.Square, self.src_reads(t), R_JUNK + ["ss%d" % col], scale=1.0 / 32.0,
                    accum_out=ss[:, col:col + 1], tag=self.tagp + "sq")
                act(sd[:, col:col + 1], ss[:, col:col + 1], AF.Ln, ["ss%d" % col, "epsb"], ["sd%d" % col],
                    bias=epsb[:, 0:1], tag=self.tagp + "ln")
                act(rstd[:, col:col + 1], sd[:, col:col + 1], AF.Exp, ["sd%d" % col], ["rstd%d" % col],
                    scale=-0.5, tag=self.tagp + "rs")

            def _s2(self, k):
                t = self.tiles[k]
                col = self.stat_off + t
                gslot = self.gslot
                src_tile = self.src_tile
                if self.dst is None:
                    vop("dve", lambda h, t=t, col=col: h.scalar_tensor_tensor(
                        out=src_tile(t), in0=src_tile(t), scalar=rstd[:, col:col + 1], in1=gB[gslot],
                        op0=ALU.mult, op1=ALU.mult), self.src_reads(t) + ["rstd%d" % col, "gb%d" % gslot],
                        self.src_reads(t), tag=self.tagp + "out")
                    dma("sp", out_d[t * 128:(t + 1) * 128, :], src_tile(t), "outs", reads=self.src_reads(t),
                        writes=["out%d" % t], tag="store")
                    return
                hb = hbf[k % 2]
                vop("dve", lambda h, t=t, col=col, hb=hb: h.scalar_tensor_tensor(
                    out=hb, in0=src_tile(t), scalar=rstd[:, col:col + 1], in1=gB[gslot],
                    op0=ALU.mult, op1=ALU.mult),
                    self.src_reads(t) + ["rstd%d" % col, "gb%d" % gslot], r_hbf(k % 2), tag=self.tagp + "h")

            def _s3(self, k):
                hb = hbf[k % 2]
                bank = self.banks[k % len(self.banks)]
                psb = ps[bank][:, :].bitcast(BF16)

                def fn(h, hb=hb, psb=psb):
                    ins = None
                    for c in range(DC):
                        ins = h.transpose(out=psb[:, c * 128:(c + 1) * 128], in_=hb[:, c * 128:(c + 1) * 128],
                                          identity=identb[:, :])
                    return ins
                sch.add("pe", fn, reads=r_hbf(k % 2) + ["identb"], writes=r_ps(bank), tag=self.tagp + "tr")

            def _s4(self, k):
                t = self.tiles[k]
                bank = self.banks[k % len(self.banks)]
                psb = ps[bank][:, :].bitcast(BF16)
                evac(self.dst[:, :, t * 128:(t + 1) * 128], psb.rearrange("p (c k) -> p c k", c=DC),
                     r_ps(bank), self.dst_regions(t), tag=self.tagp + "trev")

            def _step(self, i):
                n = len(self.tiles)
                if self.dst is not None:
                    if 0 <= i - 3 < n:
                        self._s4(i - 3)
                    if 0 <= i - 2 < n:
                        self._s3(i - 2)
                if 0 <= i - 1 < n:
                    self._s2(i - 1)
                if 0 <= i < n:
                    self._s1(i)

            def push(self, t):
                self.tiles.append(t)
                self._step(len(self.tiles) - 1)

            def flush(self):
                n = len(self.tiles)
                for i in range(n, n + 3):
                    self._step(i)

        ns1 = NormStream(lambda t: xs[:, t, :], lambda t: r_xs(t), 0, hT, lambda t: ["hT%d" % t], 0, "n1")
        def va_proj(t):
            bank = t % 2
            mm_group(ps[bank][:, :], [(hT[:, c, t * 128:(t + 1) * 128], wVa[:, c, :]) for c in range(DC)],
                     ["hT%d" % t] + r_ws(0), r_ps(bank), tag="va_mm")
            evac(Va[:, t, :, 0:64], ps[bank][:, :].rearrange("p (h k) -> p h k", h=8), r_ps(bank), r_Va(t), tag="va_ev")
        n1_order = list(range(8, 16)) + list(range(0, 8))
        for i, t in enumerate(n1_order):
            ns1.push(t)
            if i == 10:
                vop("pool", lambda h: h.memset(Va[:, :, :, 64:128], 1.0), [], [r for t in range(NT) for r in r_Va(t)])
            if i >= 12:
                va_proj(n1_order[i - 12])
        ns1.flush()
        for i in range(4, 16):
            va_proj(n1_order[i])
        dbg_dump("hT", hT_t[:, :], ["hT%d" % t for t in range(NT)])
        dbg_dump("TT", TT_t[:, :], ["TT0", "TT1"])
        dbg_dump("rstd", rstd[:, :], ["rstd%d" % t for t in range(NT)])

        def zero_ss():
            vop("dve", lambda h: h.memset(ss[:, :], 0.0), ["ss%d" % c for c in range(18)],
                ["ss%d" % c for c in range(18)])

        wo_p = [None, None]
        dbg_dump("Va", arena[:, 16384:32768], [r for t in range(NT) for r in r_Va(t)])

        pt_ctr = {"i": 0}

        def attention_items(items, sbanks, lag, tagp="", inserts=None, pts=None, defer_steps=6):
            n = len(items)

            def do_qk(it, sb_):
                sch.add("pe", lambda h, it=it, sb_=sb_: it["qk"](h, sb_), reads=it["qk_reads"], writes=r_ps(sb_),
                        tag=tagp + "qk")
                if pts is None:
                    pb = pt_ctr["i"] % NPT
                else:
                    pb = pts[pt_ctr["i"] % len(pts)]
                pt_ctr["i"] += 1
                it["pb"] = pb
                c0, c1 = it["cols"]
                act(PT[pb][:, c0:c1], ps[sb_][:, c0:c1], AF.Exp, r_ps(sb_), r_PT(pb), tag=tagp + "exp")

            def do_pv(it):
                pb = it["pb"]
                c0, c1 = it["cols"]

                def fn(h, it=it, pb=pb, c0=c0, c1=c1):
                    ins = None
                    for (o, l, st, sp_) in it["pv"]:
                        ins = h.matmul(o[:, c0:c1], l, PT[pb][:, c0:c1], start=st, stop=sp_)
                    return ins
                wr = []
                for b_ in it["pv_banks"]:
                    wr += r_ps(b_)
                sch.add("pe", fn, reads=r_PT(pb) + it["pv_reads"], writes=wr, tag=tagp + "pv")
                if it.get("post") is not None:
                    later = it["post"]()
                    if later is not None:
                        deferred.append([defer_steps, later])

            deferred = []
            for j in range(n + lag):
                if inserts and j in inserts:
                    inserts[j]()
                for d in deferred:
                    d[0] -= 1
                for d in [d for d in deferred if d[0] <= 0]:
                    d[1]()
                    deferred.remove(d)
                if j < n:
                    do_qk(items[j], sbanks[j % len(sbanks)])
                if j - lag >= 0:
                    do_pv(items[j - lag])
            for d in deferred:
                d[1]()

        MSETS = [(0, 2), (1, 3), (4, 5)]

        def moba_set(h):
            return MSETS[h % 3]

        moba_w = {0: pre_sub}

        def moba_load_w(hp):
            subq = (hp % 2) * 2
            moba_w[hp] = (load_sub(subq, w_in_d, hp * 128, tag="wqa"),
                          load_sub(subq + 1, w_in_d, 512 + hp * 128, tag="wka"))

        def moba_proj_group(hp, which, qc):
            subq = (hp % 2) * 2
            w = moba_w[hp][which]
            bank = 4 + (qc % 2)
            mm_group(ps[bank][:, :], [(w[:, c, :], hT[:, c, qc * 512:(qc + 1) * 512]) for c in range(DC)],
                     r_hT(4 * qc, 4 * qc + 4) + ["wsub%d" % (subq + which)], r_ps(bank), tag="pa_mm")
            for hh in range(2):
                slot = moba_set(2 * hp + hh)[which]
                evac(QK[slot][0:64, qc * 512:(qc + 1) * 512], ps[bank][hh * 64:(hh + 1) * 64, :], r_ps(bank),
                     r_QK(slot, qc), scale=(0.125 if which == 0 else None), eng="dve", tag="pa_ev")

        def moba_prep_k(h):
            hh = h % 2
            qs, ks = moba_set(h)
            Bk = QK[ks]
            vop("dve", lambda hd, Bk=Bk: hd.tensor_reduce(
                out=kmf[0:64, 0:8], in_=Bk[0:64, :].rearrange("p (j l) -> p j l", j=8), axis=AX.X, op=ALU.add),
                r_QK(ks), ["kmf"], tag="kmean")
            vop("dve", lambda hd, hh=hh: hd.tensor_scalar(
                out=kmb4[0:64, hh, 0, :], in0=kmf[0:64, 0:8], scalar1=1.0 / 256.0, scalar2=None, op0=ALU.mult),
                ["kmf"], ["kmb%d" % hh], tag="kmhi")
            vop("dve", lambda hd, hh=hh: hd.scalar_tensor_tensor(
                out=kmb4[0:64, hh, 1, :], in0=kmf[0:64, 0:8], scalar=1.0 / 256.0, in1=kmb4[0:64, hh, 0, :],
                op0=ALU.mult, op1=ALU.subtract), ["kmf", "kmb%d" % hh], ["kmb%d" % hh], tag="kmlo")

        def moba_prep_gate(h):
            hh = h % 2
            qs, ks = moba_set(h)
            Aq = QK[qs]

            def gate_fn(hd, Aq=Aq, hh=hh):
                ins = None
                for qt in range(16):
                    hd.matmul(ps[7][:, qt * 8:(qt + 1) * 8], Aq[0:72, qt * 128:(qt + 1) * 128], kmb4[0:72, hh, 0, :],
                              start=True, stop=False)
                    ins = hd.matmul(ps[7][:, qt * 8:(qt + 1) * 8], Aq[0:72, qt * 128:(qt + 1) * 128],
                                    kmb4[0:72, hh, 1, :], start=False, stop=True)
                return ins
            sch.add("pe", gate_fn, reads=r_QK(qs) + ["kmb%d" % hh], writes=r_ps(7), tag="gate")
            vop("dve", lambda hd: hd.tensor_tensor(out=g2[:, :], in0=ps[7][:, 0:128], in1=negmask[:, :], op=ALU.add),
                r_ps(7) + ["negmask"], ["g2"], tag="g2")

            def top_fn(hd):
                ins = None
                for qt in range(16):
                    ins = hd.max(out=top8[:, qt * 8:(qt + 1) * 8], in_=g2[:, qt * 8:(qt + 1) * 8])
                return ins
            vop("dve", top_fn, ["g2"], ["top8"], tag="top8")
            vop("dve", lambda hd: hd.tensor_tensor(
                out=selt[:, :].rearrange("p (t j) -> p t j", t=16),
                in0=g2[:, :].rearrange("p (t j) -> p t j", t=16),
                in1=top8[:, :].rearrange("p (t j) -> p t j", t=16)[:, :, 2:3].broadcast_to([128, 16, 8]),
                op=ALU.is_ge), ["g2", "top8"], ["selt"], tag="sel")
            vop("dve", lambda hd: hd.scalar_tensor_tensor(
                out=mpad3[:, :, 64:72], in0=selt[:, :].rearrange("p (t j) -> p t j", t=16), scalar=-1.0,
                in1=ownm1[:, :].rearrange("p (t j) -> p t j", t=16), op0=ALU.add, op1=ALU.max),
                ["selt", "ownm1"], ["mpad"], tag="mval")

        def moba_prep_mask(h):
            qs, ks = moba_set(h)
            Aq = QK[qs]
            for qc in range(4):
                def mt_fn(hd, qc=qc):
                    ins = None
                    for j in range(4):
                        qt = qc * 4 + j
                        ins = hd.matmul(ps[7][0:72, j * 128:(j + 1) * 128], mpad3[:, qt, :], I3[:, :],
                                        start=True, stop=True)
                    return ins
                sch.add("pe", mt_fn, reads=["mpad", "I3"], writes=r_ps(7), tag="mtr")
                evac(Aq[64:72, qc * 512:(qc + 1) * 512], ps[7][64:72, :], r_ps(7), r_QK(qs, qc), eng="dve", tag="mtr_ev")

        def moba_items(h):
            hp, hh = h // 2, h % 2
            qs, ks = moba_set(h)
            Aq, Bk = QK[qs], QK[ks]
            items = []
            for qc in range(4):
                obank = 2 + (qc % 2)
                nkt = 4 * qc + 4
                for kt in range(nkt):
                    i_d = kt - 4 * qc
                    c0 = 128 * i_d if i_d > 0 else 0
                    adds = []
                    for j in range(4):
                        if 128 * j < c0:
                            continue
                        if kt == 4 * qc + j:
                            adds.append((j, 0))
                        elif kt == 4 * qc + j - 1:
                            adds.append((j, 1))

                    def qk_fn(hd, sbank, Aq=Aq, Bk=Bk, qc=qc, kt=kt, c0=c0, adds=adds, h=h):
                        ins = hd.matmul(ps[sbank][:, c0:512], Bk[0:72, kt * 128:(kt + 1) * 128],
                                        Aq[0:72, qc * 512 + c0:(qc + 1) * 512], start=True, stop=(len(adds) == 0))
                        for ai, (j, tsel) in enumerate(adds):
                            ins = hd.matmul(ps[sbank][:, j * 128:(j + 1) * 128], Jb[:, :], TT[:, h, tsel, :],
                                            start=False, stop=(ai == len(adds) - 1))
                        return ins
                    it = dict(qk=qk_fn, qk_reads=r_QK(qs, qc) + r_QK(ks, kt // 4) + ["TT0", "TT1", "Jb"],
                              cols=(c0, 512),
                              pv=[(ps[obank], Va[:, kt, h, :], kt == 0, kt == nkt - 1)],
                              pv_reads=r_Va(kt), pv_banks=[obank], post=None)
                    if kt == nkt - 1:
                        def post(hh=hh, hp=hp, qc=qc, obank=obank):
                            rc = rcb[qc % 2]
                            rrc = ["rcb%d" % (qc % 2)]
                            lo = 64 * hh
                            act(rc[lo:lo + 64, :], ps[obank][64:128, :], AF.Ln, r_ps(obank), rrc, tag="rc_ln")
                            act(rc[lo:lo + 64, :], rc[lo:lo + 64, :], AF.Exp, rrc, rrc, scale=-1.0, tag="rc")
                            vop("dve", lambda hd: hd.tensor_tensor(
                                out=mixT[lo:lo + 64, 4 * qc:4 * qc + 4, hp, :],
                                in0=ps[obank][0:64, :].rearrange("p (t k) -> p t k", t=4),
                                in1=rc[lo:lo + 64, :].rearrange("p (t k) -> p t k", t=4), op=ALU.mult),
                                r_ps(obank) + rrc, [r for t in range(4 * qc, 4 * qc + 4) for r in r_mixT(t)],
                                tag="onorm")
                        it["post"] = post
                    items.append(it)
            return items

        vop("pool", lambda h: h.memset(QK[4][64:72, :], 0.0), [], r_QK(4))
        MOBA_SB = [0, 1, 6]
        MOBA_LAG = 3
        diff_w = {}

        def diff_load_w(hb):
            subq = (hb % 2) * 2
            diff_w[hb] = (load_sub(subq, w_in_d, 1536 + hb * 128, tag="wqb"),
                          load_sub(subq + 1, w_in_d, 2048 + hb * 128, tag="wkb"))

        def diff_proj_group(hb, which, qc, bank=6):
            sl = hb % 2
            subq = (hb % 2) * 2
            w = diff_w[hb][which]
            mm_group(ps[bank][:, :], [(w[:, c, :], hT[:, c, qc * 512:(qc + 1) * 512]) for c in range(DC)],
                     r_hT(4 * qc, 4 * qc + 4) + ["wsub%d" % (subq + which)], r_ps(bank), tag="pb_mm")
            if which == 0:
                evac(QK[sl][:, qc * 512:(qc + 1) * 512], ps[bank][:, :], r_ps(bank), r_QK(sl, qc), scale=0.125,
                     eng="dve", tag="qb_ev")
            else:
                evac(QK[2 + sl][0:64, qc * 512:(qc + 1) * 512], ps[bank][0:64, :], r_ps(bank), r_QK(2 + sl, qc),
                     eng="dve", tag="kb_ev")
                evac(QK[4 + sl][64:128, qc * 512:(qc + 1) * 512], ps[bank][64:128, :], r_ps(bank), r_QK(4 + sl, qc),
                     eng="dve", tag="kb_ev")

        nsm = NormStream(lambda t: xtmp[t], lambda t: ["xtmp%d" % t], 1, mTp, lambda t: ["mTp"], 16, "nm", banks=(4, 5))

        def kc_group(ec):
            wp = kvw["p"]
            bank = 4 + ec % 2
            mm_group(ps[bank][:, 0:256], [(wp[:, c, (ec % 4) * 128:(ec % 4 + 1) * 128], mTp[:, c, :]) for c in range(8)],
                     ["mTp"] + r_ws(3), r_ps(bank), tag="kc_mm")
            evac(KcT2[:, ec, :], ps[bank][:, 0:256], r_ps(bank), ["xtmp0"], eng="dve", tag="kc_ev")

        def vc_group(mt, half):
            wp = kvw["p"]
            bank = 4 + mt % 2
            mm_group(ps[bank][:, :], [(mTp[:, c, mt * 128:(mt + 1) * 128], wp[:, c, :]) for c in range(8)],
                     ["mTp"] + r_ws(3), r_ps(bank), tag="vc_mm")
            evac(Vc2[:, mt, half * 512:(half + 1) * 512], ps[bank][:, :], r_ps(bank), ["xtmp1"], eng="dve", tag="vc_ev")

        def kv_inserts(hp):
            ins = {}
            if hp == 0:
                ins[3] = (lambda: nsm.push(0))
                ins[5] = (lambda: nsm.push(1))
                ins[7] = (lambda: nsm._step(2))
                ins[9] = (lambda: nsm._step(3))
                ins[11] = (lambda: (nsm._step(4),
                                    dma("sp", gB[1], g_cross_d.partition_broadcast(128), "gb1", writes=["gb1"])))
                for i, ec in enumerate(range(0, 4)):
                    ins[17 + 4 * i] = (lambda ec=ec: kc_group(ec))
                ins[34] = (lambda: kvw.__setitem__("p", load_panel(3, w_ck_d, 512, 512, tag="wck1")))
            elif hp == 1:
                for i, ec in enumerate(range(4, 8)):
                    ins[17 + 4 * i] = (lambda ec=ec: kc_group(ec))
                ins[34] = (lambda: kvw.__setitem__("p", load_panel(3, w_cv_d, 0, 512, tag="wcv0")))
            elif hp == 2:
                ins[17] = (lambda: vc_group(0, 0))
                ins[25] = (lambda: vc_group(1, 0))
                ins[34] = (lambda: kvw.__setitem__("p", load_panel(3, w_cv_d, 512, 512, tag="wcv1")))
            else:
                ins[17] = (lambda: vc_group(0, 1))
                ins[25] = (lambda: vc_group(1, 1))
                ins[34] = (lambda: wo_p.__setitem__(0, load_panel(3, w_out_d, 0, 512, tag="wo0")))
            return ins

        for which in range(2):
            for qc in range(4):
                moba_proj_group(0, which, qc)
        moba_prep_k(0)
        moba_prep_gate(0)
        moba_prep_mask(0)
        for hp in range(4):
            hA, hB = 2 * hp, 2 * hp + 1
            itemsA = moba_items(hA)
            itemsB = moba_items(hB)
            insA = {0: (lambda hB=hB: moba_prep_k(hB)), 6: (lambda hB=hB: moba_prep_gate(hB)),
                    14: (lambda hB=hB: moba_prep_mask(hB))}
            if hp == 0:
                def _wo():
                    wo_p[1] = load_panel(0, w_out_d, 512, 512, tag="wo1")
                insA[2] = _wo
            if hp + 1 < 4:
                insA[4] = (lambda hp=hp: moba_load_w(hp + 1))
            else:
                insA[4] = (lambda: diff_load_w(0))
            for k_, v_ in kv_inserts(hp).items():
                assert k_ not in insA
                insA[k_] = v_
            attention_items(itemsA, MOBA_SB, MOBA_LAG, tagp="moba", inserts=insA)
            insB = {}
            if hp == 3:
                def _diff_pre():
                    vop("pool", lambda h: h.memset(QK[2][64:128, :], 0.0), [], r_QK(2))
                    vop("pool", lambda h: h.memset(QK[4][0:64, :], 0.0), [], r_QK(4))
                insB[1] = _diff_pre
                k = 0
                for which in range(2):
                    for qc in range(4):
                        insB[5 + 4 * k] = (lambda which=which, qc=qc: diff_proj_group(0, which, qc, bank=4 + (qc % 2)))
                        k += 1
            if hp + 1 < 4:
                k = 0
                for which in (1, 0):
                    for qc in range(4):
                        insB[3 + 4 * k] = (lambda hp=hp, which=which, qc=qc: moba_proj_group(hp + 1, which, qc))
                        k += 1
                insB[20] = (lambda hp=hp: moba_prep_k(2 * hp + 2))
                insB[36] = (lambda hp=hp: moba_prep_gate(2 * hp + 2))
                insB[42] = (lambda hp=hp: moba_prep_mask(2 * hp + 2))
            attention_items(itemsB, MOBA_SB, MOBA_LAG, tagp="moba", inserts=insB)

        dbg_dump("mixT_raw", arena[:, 0:16384], [r for t in range(NT) for r in r_mixT(t)])
        for qc in range(4):
            mreg = [r for t in range(4 * qc, 4 * qc + 4) for r in r_mixT(t)]
            for c in range(4):
                act(osq.rearrange("p (t k) -> p t k", t=4), mixT[:, 4 * qc:4 * qc + 4, c, :], AF.Square, mreg, R_OSQ,
                    tag="msq")
                mm_group(ps[7][:, :], [(ones_b[:, :], osq)], R_OSQ + ["ones_b"], r_ps(7),
                         start_first=(c == 0), stop_last=(c == 3), tag="msum")
            rc = rcb[qc % 2]
            rrc = ["rcb%d" % (qc % 2)]
            act(rc, ps[7][:, :], AF.Ln, r_ps(7) + ["epsb"], rrc, scale=1.0 / 512.0, bias=epsb[:, 0:1], tag="mln")
            act(rc, rc, AF.Exp, rrc, rrc, scale=-0.5, tag="mrs")
            for c in range(4):
                vop("dve", lambda hd, rc=rc, c=c, qc=qc: hd.scalar_tensor_tensor(
                    out=mixT[:, 4 * qc:4 * qc + 4, c, :], in0=mixT[:, 4 * qc:4 * qc + 4, c, :], scalar=gA[:, c:c + 1],
                    in1=rc.rearrange("p (t k) -> p t k", t=4), op0=ALU.mult, op1=ALU.mult),
                    mreg + rrc + ["gA"], mreg, tag="mscale")

        for t in range(NT):
            bank = 4 + (t % 2)
            mm_group(ps[bank][:, :], [(hT[:, c, t * 128:(t + 1) * 128], wVb[:, c, :]) for c in range(DC)],
                     ["hT%d" % t] + r_ws(1), r_ps(bank), tag="vb_mm")
            evac(Vb[:, t, :, :], ps[bank][:, :].rearrange("p (h k) -> p h k", h=4), r_ps(bank), r_Vb(t), tag="vb_ev")

        vop("pool", lambda h: h.memset(QK[3][64:128, :], 0.0), [], r_QK(3))
        vop("pool", lambda h: h.memset(QK[5][0:64, :], 0.0), [], r_QK(5))

        def diff_items(hb):
            h = 8 + hb
            sl = hb % 2
            Aq = QK[sl]
            items = []
            for qc in range(4):
                nkt = 4 * qc + 4
                for kt in range(nkt):
                    i_d = kt - 4 * qc
                    c0 = 128 * i_d if i_d > 0 else 0
                    adds = []
                    for j in range(4):
                        if 128 * j < c0:
                            continue
                        if kt == 4 * qc + j:
                            adds.append((j, 0))
                        elif kt == 4 * qc + j - 1:
                            adds.append((j, 1))
                    for m in range(2):
                        Km = QK[2 + 2 * m + sl]

                        def qk_fn(hd, sbank, Aq=Aq, Km=Km, qc=qc, kt=kt, c0=c0, adds=adds, h=h):
                            ins = hd.matmul(ps[sbank][:, c0:512], Km[:, kt * 128:(kt + 1) * 128],
                                            Aq[:, qc * 512 + c0:(qc + 1) * 512], start=True, stop=(len(adds) == 0))
                            for ai, (j, tsel) in enumerate(adds):
                                ins = hd.matmul(ps[sbank][:, j * 128:(j + 1) * 128], Jb[:, :], TT[:, h, tsel, :],
                                                start=False, stop=(ai == len(adds) - 1))
                            return ins
                        it = dict(qk=qk_fn, qk_reads=r_QK(sl, qc) + r_QK(2 + 2 * m + sl, kt // 4) + ["TT0", "TT1", "Jb"],
                                  cols=(c0, 512),
                                  pv=[(ps[2 + m], Vb[:, kt, hb, :], kt == 0, kt == nkt - 1),
                                      (ps[4 + m], ones_b[:, :], kt == 0, kt == nkt - 1)],
                                  pv_reads=r_Vb(kt) + ["ones_b"], pv_banks=[2 + m, 4 + m], post=None)
                        if kt == nkt - 1 and m == 1:
                            def post(hb=hb, qc=qc):
                                r0, r1, s0, s1 = rcb[0], rcb[1], rcb[2], rcb[3]
                                mreg = [r for t in range(4 * qc, 4 * qc + 4) for r in r_mixT(t)]
                                vop("dve", lambda hd: hd.tensor_copy(out=r0, in_=ps[2][:, :]), r_ps(2), ["rcb0"], tag="d_c0")
                                vop("dve", lambda hd: hd.tensor_copy(out=r1, in_=ps[3][:, :]), r_ps(3), ["rcb1"], tag="d_c1")
                                act(s0, ps[4][:, :], AF.Ln, r_ps(4), ["rcb2"], tag="d_ln0")
                                act(s1, ps[5][:, :], AF.Ln, r_ps(5), ["rcb3"], tag="d_ln1")
                                act(s0, s0, AF.Exp, ["rcb2"], ["rcb2"], scale=-1.0, tag="d_r0")
                                act(s1, s1, AF.Exp, ["rcb3"], ["rcb3"], scale=-1.0, tag="d_r1")
                                vop("dve", lambda hd: hd.tensor_tensor(out=r0, in0=r0, in1=s0, op=ALU.mult),
                                    ["rcb0", "rcb2"], ["rcb0"], tag="d_a0")
                                vop("dve", lambda hd: hd.tensor_tensor(out=r1, in0=r1, in1=s1, op=ALU.mult),
                                    ["rcb1", "rcb3"], ["rcb1"], tag="d_a1")
                                vop("dve", lambda hd: hd.scalar_tensor_tensor(
                                    out=r0, in0=r1, scalar=neglam, in1=r0, op0=ALU.mult, op1=ALU.add),
                                    ["rcb0", "rcb1", "lams"], ["rcb0"], tag="d_o")
                                vop("dve", lambda hd: hd.tensor_tensor(out=osq, in0=r0, in1=r0, op=ALU.mult),
                                    ["rcb0"], R_OSQ, tag="d_sq")

                                def post_b(hb=hb, qc=qc, r0=r0, s0=s0, mreg=mreg):
                                    mm_group(ps[6][:, :], [(ones_b[:, :], osq)], R_OSQ + ["ones_b"], r_ps(6), tag="d_ss")
                                    act(s0, ps[6][:, :], AF.Ln, r_ps(6) + ["epsb"], ["rcb2"], scale=1.0 / 128.0,
                                        bias=epsb[:, 0:1], tag="d_lnss")
                                    act(s0, s0, AF.Exp, ["rcb2"], ["rcb2"], scale=-0.5, tag="d_rs")
                                    vop("dve", lambda hd: hd.scalar_tensor_tensor(
                                        out=mixT[:, 4 * qc:4 * qc + 4, 4 + hb, :],
                                        in0=r0.rearrange("p (t k) -> p t k", t=4), scalar=gS8[:, 0:1],
                                        in1=s0.rearrange("p (t k) -> p t k", t=4), op0=ALU.mult, op1=ALU.mult),
                                        ["rcb0", "rcb2", "gS8"], mreg, tag="d_out")
                                return post_b
                            it["post"] = post
                        items.append(it)
            return items

        DIFF_SB = [0, 1, 7]
        DIFF_LAG = 3
        for hb in range(4):
            items = diff_items(hb)
            ins = {}
            if hb + 1 < 4:
                ins[2] = (lambda hb=hb: diff_load_w(hb + 1))
                k = 0
                for which in range(2):
                    for qc in range(4):
                        ins[12 + 8 * k] = (lambda hb=hb, which=which, qc=qc: diff_proj_group(hb + 1, which, qc))
                        k += 1
            attention_items(items, DIFF_SB, DIFF_LAG, tagp="diff", inserts=ins)

        dbg_dump("mixT", arena[:, 0:16384], [r for t in range(NT) for r in r_mixT(t)])
        dma("sp", gB[0], g_ffn_d.partition_broadcast(128), "gb0", writes=["gb0"])
        zero_ss()
        ns2 = NormStream(lambda t: xs[:, t, :], lambda t: r_xs(t), 1, hT, lambda t: ["hT%d" % t], 0, "n2")
        order = list(range(8, 16)) + list(range(7, -1, -1))
        for i, t in enumerate(order):
            xb = i % 2
            r_xre = r_a2(xb * 2048, (xb + 1) * 2048)
            dma("sp", xre[xb], x_d[t * 128:(t + 1) * 128, :], "xre%d" % xb, writes=r_xre, tag="xre")
            for half in range(2):
                bank = (2 * i + half) % 4
                mm_group(ps[bank][:, :], [(mixT[:, t, c, :], wo_p[half][:, c, :]) for c in range(8)],
                         r_mixT(t) + r_ws(3 if half == 0 else 0), r_ps(bank), tag="wo_mm")
            for half in range(2):
                bank = (2 * i + half) % 4
                vop("dve", lambda hd, t=t, half=half, bank=bank, xb=xb: hd.tensor_tensor(
                    out=xs[:, t, half * 512:(half + 1) * 512], in0=ps[bank][:, :],
                    in1=xre[xb][:, half * 512:(half + 1) * 512], op=ALU.add),
                    r_ps(bank) + r_xre, r_xs(t), tag="wo_ev")
            if STOP_AFTER != "mix":
                ns2.push(t)
        if STOP_AFTER != "mix":
            ns2.flush()

        def final_out_only():
            pass

        def phase_cross():
            QcA = a2[:, 8192:12288].rearrange("p (c k) -> p c k", c=8)

            def r_qca(g):
                return r_a2(8192 + g * 512, 8192 + (g + 1) * 512)
            dma("sp", gB[1], g_fin_d.partition_broadcast(128), "gb1", writes=["gb1"])
            wcq = [load_panel(1, w_cq_d, 0, 512, tag="wcq0"), load_panel(2, w_cq_d, 512, 512, tag="wcq1")]
            wco = [load_panel(3, w_co_d, 0, 512, tag="wco0"), load_panel(0, w_co_d, 512, 512, tag="wco1")]
            rwcq = [r_ws(1), r_ws(2)]
            rwco = [r_ws(3), r_ws(0)]
            zero_ss()
            ns3 = NormStream(lambda t: xs[:, t, :], lambda t: r_xs(t), 0, hT, lambda t: ["hT%d" % t], 0, "n3", banks=(7,))

            def qproj(qc, g):
                hd_, e2 = g // 2, g % 2
                bank = 5 + (g % 2)
                col = ((hd_ % 2) * 2 + e2) * 128
                mm_group(ps[bank][:, :], [(wcq[hd_ // 2][:, c, col:col + 128], hT[:, c, qc * 512:(qc + 1) * 512])
                                          for c in range(8)],
                         r_hT(4 * qc, 4 * qc + 4) + rwcq[hd_ // 2], r_ps(bank), tag="qc_mm")
                evac(QcA[:, g, :], ps[bank][:, :], r_ps(bank), r_qca(g), scale=1.0 / 16.0, tag="qc_ev")

            for g in range(8):
                qproj(0, g)
            for qc in range(4):
                ob = qc % 2
                r_oc = r_a2(ob * 4096, (ob + 1) * 4096)
                items = []
                for hd_ in range(4):
                    for mt in range(2):
                        def qk_fn(hd, sbank, hd_=hd_, mt=mt):
                            ins = None
                            for e2 in range(2):
                                ins = hd.matmul(ps[sbank][:, :], KcT2[:, 2 * hd_ + e2, mt * 128:(mt + 1) * 128],
                                                QcA[:, 2 * hd_ + e2, :], start=(e2 == 0), stop=(e2 == 1))
                            return ins
                        it = dict(qk=qk_fn, qk_reads=r_qca(2 * hd_) + r_qca(2 * hd_ + 1) + ["xtmp0"], cols=(0, 512),
                                  pv=[(ps[2 + dv2], Vc2[:, mt, (2 * hd_ + dv2) * 128:(2 * hd_ + dv2 + 1) * 128], mt == 0, mt == 1)
                                      for dv2 in range(2)] + [(ps[4], ones_b[:, :], mt == 0, mt == 1)],
                                  pv_reads=["xtmp1", "ones_b"], pv_banks=[2, 3, 4], post=None)
                        if mt == 1:
                            def post(hd_=hd_, ob=ob, r_oc=r_oc):
                                c0, c1, rc = rcb[0], rcb[1], rcb[3]
                                vop("dve", lambda hd: hd.tensor_copy(out=c0, in_=ps[2][:, :]), r_ps(2), ["rcb0"], tag="c_c0")
                                vop("dve", lambda hd: hd.tensor_copy(out=c1, in_=ps[3][:, :]), r_ps(3), ["rcb1"], tag="c_c1")
                                act(rc, ps[4][:, :], AF.Ln, r_ps(4), ["rcb3"], tag="c_ln")
                                act(rc, rc, AF.Exp, ["rcb3"], ["rcb3"], scale=-1.0, tag="c_rc")
                                vop("dve", lambda hd: hd.tensor_tensor(out=ocT[ob][:, 2 * hd_, :], in0=c0, in1=rc, op=ALU.mult),
                                    ["rcb0", "rcb3"], r_oc, tag="c_on")
                                vop("dve", lambda hd: hd.tensor_tensor(out=ocT[ob][:, 2 * hd_ + 1, :], in0=c1, in1=rc, op=ALU.mult),
                                    ["rcb1", "rcb3"], r_oc, tag="c_on")
                            it["post"] = post
                        items.append(it)
                ins = {}
                if qc + 1 < 4:
                    for hd_ in range(4):
                        ins[2 * hd_ + 2] = (lambda qc=qc, hd_=hd_: (qproj(qc + 1, 2 * hd_), qproj(qc + 1, 2 * hd_ + 1)))
                attention_items(items, [0, 1], 2, tagp="cross", inserts=ins, pts=[4, 5, 6])
                for tt in range(4):
                    t = 4 * qc + tt
                    for half in range(2):
                        bank = 5 + half
                        mm_group(ps[bank][:, :], [(ocT[ob][:, c, tt * 128:(tt + 1) * 128], wco[half][:, c, :]) for c in range(8)],
                                 r_oc + rwco[half], r_ps(bank), tag="co_mm")
                        vop("dve", lambda hd, t=t, half=half, bank=bank: hd.tensor_tensor(
                            out=xs[:, t, half * 512:(half + 1) * 512], in0=ps[bank][:, :],
                            in1=xs[:, t, half * 512:(half + 1) * 512], op=ALU.add),
                            r_ps(bank) + r_xs(t), r_xs(t), tag="co_ev")
                    if STOP_AFTER != "cross":
                        ns3.push(t)
            if STOP_AFTER != "cross":
                ns3.flush()

        def phase_ffn():
            groups = [(2560, 2)] + [(g * 512, 4) for g in range(5)]
            panel_i = {"i": 1}

            def next_slot():
                s = panel_i["i"] % 4
                panel_i["i"] += 1
                return s
            loaded = []

            def load_group(gi):
                f0, nch = groups[gi]
                sg = next_slot()
                wg = load_panel(sg, w_gate_d, f0, nch * 128, tag="wg")
                su = next_slot()
                wu = load_panel(su, w_up_d, f0, nch * 128, tag="wu")
                sd_ = next_slot()
                src = w_down_d[f0:f0 + nch * 128, :].rearrange("(c p) n -> p c n", p=128)
                dst = wsl[sd_][:, 0:nch * 1024].rearrange("p (c n) -> p c n", c=nch)
                dma("pool", dst, src, "ws%d" % sd_, writes=r_ws(sd_), tag="wd")
                loaded.append((wg, r_ws(sg), wu, r_ws(su), dst, r_ws(sd_)))
            load_group(0)
            zero_ss()
            nsf = NormStream(lambda t: xs[:, t, :], lambda t: r_xs(t), 1, None, None, 0, "nf")
            for gi, (f0, nch) in enumerate(groups):
                wg, rwg, wu, rwu, wd, rwd = loaded[gi]
                ab = gi % 2
                r_act = r_a2(ab * 8192, (ab + 1) * 8192)
                for fc in range(nch):
                    for qc in range(4):
                        bg = (2 * qc) % 4
                        bu = bg + 1
                        mm_group(ps[bg][:, :], [(wg[:, c, fc * 128:(fc + 1) * 128], hT[:, c, qc * 512:(qc + 1) * 512])
                                                for c in range(8)], r_hT(4 * qc, 4 * qc + 4) + rwg, r_ps(bg), tag="g_mm")
                        mm_group(ps[bu][:, :], [(wu[:, c, fc * 128:(fc + 1) * 128], hT[:, c, qc * 512:(qc + 1) * 512])
                                                for c in range(8)], r_hT(4 * qc, 4 * qc + 4) + rwu, r_ps(bu), tag="u_mm")
                        sgb = rcb[qc % 2]
                        rsg = ["rcb%d" % (qc % 2)]
                        act(sgb, ps[bg][:, :], AF.Silu, r_ps(bg), rsg, tag="silu")
                        vop("dve", lambda hd, ab=ab, fc=fc, qc=qc, bu=bu, sgb=sgb: hd.tensor_tensor(
                            out=actT[ab][:, fc, qc * 512:(qc + 1) * 512], in0=ps[bu][:, :], in1=sgb, op=ALU.mult),
                            r_ps(bu) + rsg, r_act, tag="gu")
                if gi + 1 < len(groups):
                    load_group(gi + 1)
                for t in range(NT):
                    for half in range(2):
                        bank = 4 + (2 * t + half) % 4
                        mm_group(ps[bank][:, :], [(actT[ab][:, fc, t * 128:(t + 1) * 128], wd[:, fc, half * 512:(half + 1) * 512])
                                                  for fc in range(nch)], r_act + rwd, r_ps(bank), tag="d_mm")
                        vop("dve", lambda hd, t=t, half=half, bank=bank: hd.tensor_tensor(
                            out=xs[:, t, half * 512:(half + 1) * 512], in0=ps[bank][:, :],
                            in1=xs[:, t, half * 512:(half + 1) * 512], op=ALU.add),
                            r_ps(bank) + r_xs(t), r_xs(t), tag="d_ev")
                    if gi == len(groups) - 1:
                        nsf.push(t)
            nsf.flush()
            sch.add("sp", lambda h: None, reads=["out%d" % t for t in range(NT)] + ["dbgout_" + n for n in dbg_d],
                    writes=[], tag="final")

        def phase_final(do_norm=True):
            if do_norm:
                dma("sp", gB[1], g_fin_d.partition_broadcast(128), "gb1", writes=["gb1"])
                zero_ss()
            outs = []
            for t in range(NT):
                if do_norm:
                    col = t
                    act(junk, xs[:, t, :], AF.Square, r_xs(t), R_JUNK + ["ss%d" % col], scale=1.0 / 32.0,
                        accum_out=ss[:, col:col + 1], tag="fsq")
                    act(sd[:, col:col + 1], ss[:, col:col + 1], AF.Sqrt, ["ss%d" % col, "epsb"], ["sd%d" % col],
                        bias=epsb[:, 0:1], tag="fsqrt")
                    vop("dve", lambda h, col=col: h.reciprocal(out=rstd[:, col:col + 1], in_=sd[:, col:col + 1]),
                        ["sd%d" % col], ["rstd%d" % col])
                    vop("dve", lambda h, t=t, col=col: h.scalar_tensor_tensor(
                        out=xs[:, t, :], in0=xs[:, t, :], scalar=rstd[:, col:col + 1], in1=gB[1],
                        op0=ALU.mult, op1=ALU.mult), r_xs(t) + ["rstd%d" % col, "gb1"], r_xs(t), tag="fout")
                outs.append(dma("sp", out_d[t * 128:(t + 1) * 128, :], xs[:, t, :], "outs", reads=r_xs(t),
                                writes=["out%d" % t], tag="store"))
            op = sch.add("sp", lambda h: None, reads=["out%d" % t for t in range(NT)] +
                         ["dbgout_" + n for n in dbg_d], writes=[], tag="final")

        if STOP_AFTER == "mix":
            phase_final(do_norm=False)
        elif STOP_AFTER == "cross":
            phase_cross()
            phase_final(do_norm=False)
        else:
            phase_cross()
            phase_ffn()

        sch.finalize()

        @block.sync
        def _(h):
            sch.emit("sp", h, semh)

        @block.tensor
        def _(h):
            sch.emit("pe", h, semh)

        @block.scalar
        def _(h):
            sch.emit("act", h, semh)

        @block.vector
        def _(h):
            sch.emit("dve", h, semh)

        @block.gpsimd
        def _(h):
            sch.emit("pool", h, semh)

    return nc


_CACHE = {}


def kernel(**inputs):
    consts = _host_consts()
    f32 = lambda a: np.ascontiguousarray(np.asarray(a, dtype=np.float32))
    shared = {
        "mix_norm_g": f32(inputs["mix_norm_g"]).reshape(D),
        "w_in": f32(inputs["w_in"]).reshape(D, 3072),
        "moba_out_g": np.ascontiguousarray(f32(inputs["moba_out_g"]).reshape(4, 128).T),
        "diff_lambda": f32(inputs["diff_lambda"]).reshape(256),
        "diff_subln_g": f32(inputs["diff_subln_g"]).reshape(128, 1),
        "w_out": f32(inputs["w_out"]).reshape(D, D),
        "rel_bias_table": f32(inputs["rel_bias_table"]).reshape(32, 12),
        "cross_norm_g": f32(inputs["cross_norm_g"]).reshape(D),
        "mem_norm_g": f32(inputs["mem_norm_g"]).reshape(D),
        "w_cq": f32(inputs["w_cq"]).reshape(D, D),
        "w_ck": f32(inputs["w_ck"]).reshape(D, D),
        "w_cv": f32(inputs["w_cv"]).reshape(D, D),
        "w_co": f32(inputs["w_co"]).reshape(D, D),
        "ffn_norm_g": f32(inputs["ffn_norm_g"]).reshape(D),
        "w_gate": f32(inputs["w_gate"]).reshape(D, DFF),
        "w_up": f32(inputs["w_up"]).reshape(D, DFF),
        "w_down": f32(inputs["w_down"]).reshape(DFF, D),
        "final_norm_g": f32(inputs["final_norm_g"]).reshape(D),
    }
    shared.update(consts)
    x = f32(inputs["x"])
    mem = f32(inputs["mem"])
    nb = x.shape[0]
    in_maps = []
    for b in range(nb):
        m = dict(shared)
        m["x"] = np.ascontiguousarray(x[b])
        m["mem"] = np.ascontiguousarray(mem[b])
        in_maps.append(m)
    nc = build_program()
    res = run_bass_kernel_spmd(nc, in_maps, core_ids=list(range(nb)))
    out = np.stack([np.asarray(r["out"], dtype=np.float32) for r in res.results], axis=0)
    if DEBUG_OUT:
        kernel.debug = [{k: np.asarray(v) for k, v in r.items() if k.startswith("dbg_")} for r in res.results]
    return out
```

```python
import math
import numpy as np
import concourse.bass as bass
import concourse.mybir as mybir
from concourse.bass_utils import run_bass_kernel_spmd

F32 = mybir.dt.float32
BF16 = mybir.dt.bfloat16
AF = mybir.ActivationFunctionType
ALU = mybir.AluOpType
AX = mybir.AxisListType

S = 2048
D = 1024
NT = 16
DC = 8
ML = 256
DFF = 2816
EPS = 1e-6
NEGM = 30000.0

DEBUG_OUT = None
STOP_AFTER = None


class _Op:
    __slots__ = ("eng", "fn", "deps", "needed", "dma_sem", "val", "tag")


class Sched:
    ENGS = ("pe", "act", "dve", "pool", "sp")

    def __init__(self):
        self.q = {e: [] for e in self.ENGS}
        self.lw = {}
        self.rd = {}

    def add(self, eng, fn, reads=(), writes=(), dma_sem=None, tag="", after=()):
        op = _Op()
        op.eng = eng
        op.fn = fn
        op.dma_sem = dma_sem
        op.needed = False
        op.val = 0
        op.tag = tag
        deps = []
        seen = set()

        def _add(d):
            if d is None or id(d) in seen:
                return
            seen.add(id(d))
            if d.dma_sem is None and d.eng == "pe" and eng == "pe" and dma_sem is None:
                return
            deps.append(d)

        for r in reads:
            _add(self.lw.get(r))
        for w in writes:
            _add(self.lw.get(w))
            for r in self.rd.get(w, ()):
                _add(r)
        for w in after:
            _add(self.lw.get(w))
            for r in self.rd.get(w, ()):
                _add(r)
        op.deps = deps
        for d in deps:
            d.needed = True
        for r in reads:
            lst = self.rd.setdefault(r, [])
            if dma_sem is None:
                lst[:] = [o for o in lst if not (o.dma_sem is None and o.eng == eng)]
            lst.append(op)
        for w in writes:
            self.lw[w] = op
            self.rd[w] = []
        self.q[eng].append(op)
        return op

    def finalize(self):
        cnt = {}
        for e in self.ENGS:
            for op in self.q[e]:
                if op.dma_sem is not None:
                    cnt[op.dma_sem] = cnt.get(op.dma_sem, 0) + 16
                    op.val = cnt[op.dma_sem]
                elif op.needed:
                    cnt[e] = cnt.get(e, 0) + 1
                    op.val = cnt[e]

    def emit(self, eng, h, semh):
        known = {}
        for op in self.q[eng]:
            waits = {}
            for d in op.deps:
                key = d.dma_sem if d.dma_sem is not None else d.eng
                if d.val > waits.get(key, 0):
                    waits[key] = d.val
            for key, val in waits.items():
                if known.get(key, 0) >= val:
                    continue
                h.wait_ge(semh[key], val)
                known[key] = val
            ins = op.fn(h)
            if op.dma_sem is not None:
                ins.then_inc(semh[op.dma_sem], 16)
            elif op.needed:
                ins.then_inc(semh[eng], 1)


def _rel_bucket_np(n):
    n = np.maximum(n, 0)
    max_exact = 16
    nf = np.maximum(n, max_exact).astype(np.float32)
    log_ratio = np.log(nf / np.float32(max_exact)) / np.float32(math.log(128 / max_exact))
    large = max_exact + (log_ratio * np.float32(32 - max_exact)).astype(np.int32)
    large = np.minimum(large, 31)
    return np.where(n < max_exact, n, large)


def _host_consts():
    c = {}
    c["c_ident"] = np.eye(128, dtype=np.float32)
    urp = np.zeros((33, 384), np.float32)
    for m in range(383):
        n = m - 127
        if n >= 0:
            b = int(_rel_bucket_np(np.array([n]))[0])
            urp[b, m] += 1.0
            urp[31, m] -= 1.0
        else:
            urp[32, m] = 1.0
    c["c_jmat"] = np.ascontiguousarray(np.eye(128, dtype=np.float32)[::-1])
    c["c_urp"] = urp
    koh = np.zeros((8, S), np.float32)
    for j in range(8):
        koh[j, j * 256:(j + 1) * 256] = 1.0
    c["c_koh"] = koh
    negmask = np.zeros((128, 16, 8), np.float32)
    ownm1 = np.full((128, 16, 8), -1.0, np.float32)
    for qt in range(16):
        own = qt // 2
        negmask[:, qt, own:] = -1e30
        ownm1[:, qt, own] = 0.0
    c["c_negmask"] = negmask.reshape(128, 128)
    c["c_ownm1"] = ownm1.reshape(128, 128)
    return c


def build_program():
    nc = bass.Bass("TRN2", target_bir_lowering=False)

    def din(name, shape):
        return nc.dram_tensor(name, list(shape), F32, kind="ExternalInput").ap()

    x_d = din("x", [S, D])
    mem_d = din("mem", [ML, D])
    g_mix_d = din("mix_norm_g", [D])
    w_in_d = din("w_in", [D, 3072])
    moba_g_d = din("moba_out_g", [128, 4])
    lam_d = din("diff_lambda", [256])
    subln_d = din("diff_subln_g", [128, 1])
    w_out_d = din("w_out", [D, D])
    tab_d = din("rel_bias_table", [32, 12])
    g_cross_d = din("cross_norm_g", [D])
    g_mem_d = din("mem_norm_g", [D])
    w_cq_d = din("w_cq", [D, D])
    w_ck_d = din("w_ck", [D, D])
    w_cv_d = din("w_cv", [D, D])
    w_co_d = din("w_co", [D, D])
    g_ffn_d = din("ffn_norm_g", [D])
    w_gate_d = din("w_gate", [D, DFF])
    w_up_d = din("w_up", [D, DFF])
    w_down_d = din("w_down", [DFF, D])
    g_fin_d = din("final_norm_g", [D])
    c_ident_d = din("c_ident", [128, 128])
    c_urp_d = din("c_urp", [33, 384])
    c_koh_d = din("c_koh", [8, S])
    c_negmask_d = din("c_negmask", [128, 128])
    c_ownm1_d = din("c_ownm1", [128, 128])
    c_jmat_d = din("c_jmat", [128, 128])
    bv_d = nc.dram_tensor("bv_scr", [12, 384], F32).ap()
    out_d = nc.dram_tensor("out", [S, D], F32, kind="ExternalOutput").ap()

    dbg_d = {}
    if DEBUG_OUT:
        for name, shape in DEBUG_OUT.items():
            dbg_d[name] = nc.dram_tensor("dbg_" + name, list(shape), F32, kind="ExternalOutput").ap()

    sch = Sched()
    sem_names = ["pe", "act", "dve", "pool"]
    sem_names += ["x%d" % t for t in range(NT)]
    sem_names += ["ws%d" % i for i in range(4)] + ["wsub%d" % i for i in range(4)]
    sem_names += ["gb0", "gb1", "xtmp0", "xtmp1", "xre0", "xre1", "outs", "dbg"]
    sem_names += ["k%d" % i for i in range(16)]

    from contextlib import ExitStack
    with ExitStack() as es:
        def sb(name, shape, dt):
            return es.enter_context(nc.sbuf_tensor(name, list(shape), dt))

        arena = sb("arena", [128, 32768], BF16)
        hT_t = sb("hT", [128, DC * S], BF16)
        a2 = sb("arena2", [128, 16384], BF16)
        ws_t = sb("ws", [128, 4 * 4096], BF16)
        TT_t = sb("TT", [128, 12 * 2 * 128], BF16)
        gB_t = sb("gB", [128, 2 * D], F32)
        xtmp_t = sb("xtmp", [128, 2 * D], F32)
        identb = sb("identb", [128, 128], BF16)
        I3 = sb("I3", [128, 128], BF16)
        ones_b = sb("ones_b", [128, 128], BF16)
        Jb = sb("Jb", [128, 128], BF16)
        bvs = sb("bvs", [12, 384], F32)
        urp = sb("urp", [33, 384], F32)
        tabx = sb("tabx", [33, 12], F32)
        negmask = sb("negmask", [128, 128], F32)
        ownm1 = sb("ownm1", [128, 128], F32)
        g2 = sb("g2", [128, 128], F32)
        top8 = sb("top8", [128, 128], F32)
        selt = sb("selt", [128, 128], F32)
        mpad = sb("mpad", [128, 16 * 72], BF16)
        kmf = sb("kmf", [128, 16], F32)
        kmb = sb("kmb", [128, 2 * 16], BF16)
        ss = sb("ss", [128, 18], F32)
        sd = sb("sd", [128, 18], F32)
        rstd = sb("rstd", [128, 18], F32)
        epsb = sb("epsb", [128, 1], F32)
        rcb_t = sb("rcb", [128, 4 * 512], F32)
        gA = sb("gA", [128, 4], F32)
        gS = sb("gS", [128, 1], F32)
        gS8 = sb("gS8", [128, 1], F32)
        mTp_t = sb("mTp", [128, 2048], BF16)
        lamt = sb("lamt", [128, 256], F32)
        lamw = sb("lamw", [128, 128], F32)
        lams = sb("lams", [128, 8], F32)

        ps = [es.enter_context(nc.psum_tensor("ps%d" % i, [128, 512], F32)) for i in range(8)]
        semh = {n: es.enter_context(nc.semaphore(n)) for n in sem_names}
        block = es.enter_context(nc.Block())

        xs = arena[:, :].bitcast(F32).rearrange("p (t d) -> p t d", t=NT)
        mixT = arena[:, 0:16384].rearrange("p (t c k) -> p t c k", t=NT, c=8)
        Va = arena[:, 16384:32768].rearrange("p (t h k) -> p t h k", t=NT, h=8)
        Vb = arena[:, 16384:24576].rearrange("p (t h k) -> p t h k", t=NT, h=4)
        hT = hT_t[:, :].rearrange("p (c s) -> p c s", c=DC)
        QK = [a2[:, i * 2048:(i + 1) * 2048] for i in range(6)]
        NPT = 7
        PT = [a2[:, 12288 + i * 512: 12288 + (i + 1) * 512] for i in range(4)] + \
             [a2[:, 14848 + i * 512: 14848 + (i + 1) * 512] for i in range(3)]
        osq = a2[:, 14336:14848]
        hbf = [a2[:, 12288 + i * 1024: 12288 + (i + 1) * 1024] for i in range(2)]
        ocT = [a2[:, i * 4096:(i + 1) * 4096].rearrange("p (c k) -> p c k", c=8) for i in range(2)]
        QcT = [a2[:, 8192 + i * 1024: 8192 + (i + 1) * 1024].rearrange("p (c k) -> p c k", c=2) for i in range(2)]
        KcT = a2[:, 10240:12288].rearrange("p (c k) -> p c k", c=8)
        Vc = a2[:, 14336:16384].rearrange("p (m k) -> p m k", m=2)
        mT = a2[:, 4096:6144].rearrange("p (c k) -> p c k", c=8)
        actT = [a2[:, i * 8192:(i + 1) * 8192].rearrange("p (c k) -> p c k", c=4) for i in range(2)]
        wsl = [ws_t[:, i * 4096:(i + 1) * 4096] for i in range(4)]
        TT = TT_t[:, :].rearrange("p (h t q) -> p h t q", h=12, t=2)
        gB = [gB_t[:, i * D:(i + 1) * D] for i in range(2)]
        xtmp = [xtmp_t[:, i * D:(i + 1) * D] for i in range(2)]
        rcb = [rcb_t[:, i * 512:(i + 1) * 512] for i in range(4)]
        junk = rcb_t[:, 1024:1536].bitcast(BF16)
        mpad3 = mpad[:, :].rearrange("p (t k) -> p t k", t=16)
        mTp = mTp_t[:, :].rearrange("p (c k) -> p c k", c=8)
        KV = xtmp_t[:, :].bitcast(BF16)
        KcT2 = KV[:, 0:2048].rearrange("p (c k) -> p c k", c=8)
        Vc2 = KV[:, 2048:4096].rearrange("p (m k) -> p m k", m=2)
        xre = [a2[:, 0:2048].bitcast(F32), a2[:, 2048:4096].bitcast(F32)]
        kmb4 = kmb[:, :].rearrange("p (b l j) -> p b l j", b=2, l=2)

        def r_xs(t):
            return ["ar%d" % (2 * t), "ar%d" % (2 * t + 1)]

        def r_mixT(t):
            return ["ar%d" % t]

        def r_Va(t):
            return ["ar%d" % (16 + t)]

        def r_Vb(t):
            return ["ar%d" % (16 + t // 2)]

        def r_a2(lo, hi):
            return ["a2_%d" % g for g in range(lo // 512, (hi + 511) // 512)]

        def r_QK(i, qc=None):
            if qc is None:
                return r_a2(i * 2048, (i + 1) * 2048)
            return r_a2(i * 2048 + qc * 512, i * 2048 + (qc + 1) * 512)

        def r_PT(i):
            if i >= 4:
                return r_a2(14848 + (i - 4) * 512, 14848 + (i - 3) * 512)
            return r_a2(12288 + i * 512, 12288 + (i + 1) * 512)

        def r_hbf(i):
            return r_a2(12288 + i * 1024, 12288 + (i + 1) * 1024)

        R_OSQ = r_a2(14336, 14848)
        R_JUNK = ["rcb2"]

        def r_ws(i):
            if i == 2:
                return ["wsub%d" % k for k in range(4)]
            return ["ws%d" % i]

        def r_hT(lo_tile, hi_tile):
            return ["hT%d" % t for t in range(lo_tile, hi_tile)]

        def r_ps(b):
            return ["ps%d" % b]

        stop = {"flag": False}

        def dma(queue, out, in_, sem, reads=(), writes=(), tag=""):
            def fn(h, out=out, in_=in_):
                return h.dma_start(out=out, in_=in_)
            return sch.add(queue, fn, reads=reads, writes=writes, dma_sem=sem, tag=tag)

        def mm_group(out, pairs, reads, writes, start_first=True, stop_last=True, tag=""):
            def fn(h, out=out, pairs=pairs):
                ins = None
                n = len(pairs)
                for i, (l, r) in enumerate(pairs):
                    ins = h.matmul(out, l, r, start=(start_first and i == 0), stop=(stop_last and i == n - 1))
                return ins
            return sch.add("pe", fn, reads=reads, writes=writes, tag=tag)

        def act(out, in_, func, reads, writes, scale=1.0, bias=None, accum_out=None, tag=""):
            def fn(h, out=out, in_=in_):
                kw = {}
                if bias is not None:
                    kw["bias"] = bias
                if accum_out is not None:
                    kw["accum_out"] = accum_out
                return h.activation(out=out, in_=in_, func=func, scale=scale, **kw)
            return sch.add("act", fn, reads=reads, writes=writes, tag=tag)

        def vop(eng, fn, reads, writes, tag=""):
            return sch.add(eng, fn, reads=reads, writes=writes, tag=tag)

        evac_rr = {"i": 0}

        def evac(out, in_, reads, writes, scale=None, eng=None, tag=""):
            if eng is None:
                eng = ("act", "dve")[evac_rr["i"] % 2]
                evac_rr["i"] += 1
            if eng == "act":
                return act(out, in_, AF.Copy, reads, writes, scale=(1.0 if scale is None else scale), tag=tag)
            if scale is None:
                return vop("dve", lambda h, out=out, in_=in_: h.tensor_copy(out=out, in_=in_), reads, writes, tag=tag)
            return vop("dve", lambda h, out=out, in_=in_: h.tensor_scalar(
                out=out, in0=in_, scalar1=float(scale), scalar2=None, op0=ALU.mult), reads, writes, tag=tag)

        def dbg_dump(name, src_ap, reads, rows=128):
            if name not in dbg_d:
                return
            dst = dbg_d[name]
            dma("pool", dst, src_ap, "dbg", reads=reads, writes=["dbgout_" + name])

        def load_panel(slot, w_d, col0, ncols, row0=0, nchunks=8, tag=""):
            src = w_d[row0:row0 + nchunks * 128, col0:col0 + ncols].rearrange("(c p) n -> p c n", p=128)
            dst = wsl[slot][:, 0:nchunks * ncols].rearrange("p (c n) -> p c n", c=nchunks)
            dma("pool", dst, src, "ws%d" % slot, writes=r_ws(slot), tag=tag)
            return dst

        def load_sub(sub, w_d, col0, tag=""):
            src = w_d[:, col0:col0 + 128].rearrange("(c p) n -> p c n", p=128)
            dst = wsl[2][:, sub * 1024:(sub + 1) * 1024].rearrange("p (c n) -> p c n", c=8)
            dma("pool", dst, src, "wsub%d" % sub, writes=["wsub%d" % sub], tag=tag)
            return dst

        dma("sp", gB[0], g_mix_d.partition_broadcast(128), "gb0", writes=["gb0"])
        dma("sp", gB[1], g_mem_d.partition_broadcast(128), "gb1", writes=["gb1"])
        dma("sp", urp[:, :], c_urp_d[:, :], "k0", writes=["urp"])
        dma("sp", tabx[0:32, :], tab_d[:, :], "k1", writes=["tabx"])
        dma("sp", negmask[:, :], c_negmask_d[:, :], "k2", writes=["negmask"])
        dma("sp", ownm1[:, :], c_ownm1_d[:, :], "k3", writes=["ownm1"])
        dma("sp", gA[:, :], moba_g_d[:, :], "k4", writes=["gA"])
        dma("sp", gS[:, :], subln_d[:, :], "k5", writes=["gS"])
        dma("sp", lamt[:, :], lam_d.partition_broadcast(128), "k6", writes=["lamt"])
        for t in list(range(8, 16)) + list(range(0, 8)):
            dma("sp", xs[:, t, :], x_d[t * 128:(t + 1) * 128, :], "x%d" % t, writes=r_xs(t), tag="xload")
        for mt in range(2):
            dma("sp", xtmp[mt], mem_d[mt * 128:(mt + 1) * 128, :], "xtmp%d" % mt, writes=["xtmp%d" % mt], tag="memld")
        dma("pool", identb[:, :], c_ident_d[:, :], "k7", writes=["identb"])
        dma("pool", Jb[:, :], c_jmat_d[:, :], "k11", writes=["Jb"])
        dma("pool", QK[2][64:72, :], c_koh_d[:, :], "k8", writes=r_QK(2))
        dma("pool", QK[3][64:72, :], c_koh_d[:, :], "k9", writes=r_QK(3))
        dma("pool", QK[5][64:72, :], c_koh_d[:, :], "k10", writes=r_QK(5))
        wVa = load_panel(0, w_in_d, 1024, 512, tag="wVa")
        wVb = load_panel(1, w_in_d, 2560, 512, tag="wVb")
        kvw = {"p": load_panel(3, w_ck_d, 0, 512, tag="wck0")}
        pre_sub = (load_sub(0, w_in_d, 0, tag="wqa"), load_sub(1, w_in_d, 512, tag="wka"))

        vop("dve", lambda h: h.memset(epsb[:, :], EPS), [], ["epsb"])
        vop("dve", lambda h: h.memset(ss[:, :], 0.0), [], ["ss%d" % c for c in range(18)])
        vop("dve", lambda h: h.memset(ones_b[:, :], 1.0), [], ["ones_b"])
        vop("dve", lambda h: h.memset(mpad[:, :], 0.0), [], ["mpad"])
        vop("dve", lambda h: h.memset(kmb[:, :], 0.0), [], ["kmb"])
        vop("dve", lambda h: h.memset(tabx[32:33, :], -NEGM), ["tabx"], ["tabx"])
        vop("pool", lambda h: h.memset(QK[0][64:72, :], 0.0), [], r_QK(0))
        vop("pool", lambda h: h.memset(QK[1][64:72, :], 0.0), [], r_QK(1))
        vop("dve", lambda h: h.tensor_scalar(out=I3[:, :], in0=identb[:, :], scalar1=NEGM, scalar2=None, op0=ALU.mult),
            ["identb"], ["I3"])
        vop("dve", lambda h: h.tensor_scalar(out=gS8[:, :], in0=gS[:, :], scalar1=0.8, scalar2=None, op0=ALU.mult),
            ["gS"], ["gS8"])

        vop("dve", lambda h: h.tensor_tensor(out=lamw[:, 0:64], in0=lamt[:, 0:64], in1=lamt[:, 64:128], op=ALU.mult),
            ["lamt"], ["lamw"])
        vop("dve", lambda h: h.tensor_tensor(out=lamw[:, 64:128], in0=lamt[:, 128:192], in1=lamt[:, 192:256], op=ALU.mult),
            ["lamt", "lamw"], ["lamw"])
        vop("dve", lambda h: h.tensor_reduce(out=lams[:, 0:2], in_=lamw[:, :].rearrange("p (a b) -> p a b", a=2),
                                              axis=AX.X, op=ALU.add), ["lamw"], ["lams"])
        act(lams[:, 2:4], lams[:, 0:2], AF.Exp, ["lams"], ["lams"])
        vop("dve", lambda h: h.scalar_tensor_tensor(out=lams[:, 4:5], in0=lams[:, 3:4], scalar=-0.2, in1=lams[:, 2:3],
                                                     op0=ALU.add, op1=ALU.subtract), ["lams"], ["lams"])
        neglam = lams[:, 4:5]

        sch.add("pe", lambda h: h.matmul(ps[0][0:12, 0:384], tabx[0:33, 0:12], urp[0:33, 0:384], start=True, stop=True),
                reads=["urp", "tabx"], writes=r_ps(0), tag="bv_mm")
        evac(bvs[:, :], ps[0][0:12, 0:384], r_ps(0), ["bvs"], eng="dve", tag="bv_ev")
        dma("sp", bv_d[:, :], bvs[:, :], "k12", reads=["bvs"], writes=["bv_dram"])
        for tsel in range(2):
            src = bass.AP(tensor=bv_d.tensor, offset=tsel * 128, ap=[[1, 128], [384, 12], [1, 128]])
            dma("pool", TT[:, :, tsel, :], src, "k%d" % (13 + tsel), reads=["bv_dram"], writes=["TT%d" % tsel])

        class NormStream:
            def __init__(self, src_tile, src_reads, gslot, dst, dst_regions, stat_off, tagp, banks=(6, 7)):
                self.src_tile, self.src_reads, self.gslot = src_tile, src_reads, gslot
                self.dst, self.dst_regions, self.stat_off, self.tagp, self.banks = dst, dst_regions, stat_off, tagp, banks
                self.tiles = []

            def _s1(self, k):
                t = self.tiles[k]
                col = self.stat_off + t
                act(junk, self.src_tile(t), AF.Square, self.src_reads(t), R_JUNK + ["ss%d" % col], scale=1.0 / 32.0,
                    accum_out=ss[:, col:col + 1], tag=self.tagp + "sq")
                act(sd[:, col:col + 1], ss[:, col:col + 1], AF.Ln, ["ss%d" % col, "epsb"], ["sd%d" % col],
                    bias=epsb[:, 0:1], tag=self.tagp + "ln")
                act(rstd[:, col:col + 1], sd[:, col:col + 1], AF.Exp, ["sd%d" % col], ["rstd%d" % col],
                    scale=-0.5, tag=self.tagp + "rs")

            def _s2(self, k):
                t = self.tiles[k]
                col = self.stat_off + t
                gslot = self.gslot
                src_tile = self.src_tile
                if self.dst is None:
                    vop("dve", lambda h, t=t, col=col: h.scalar_tensor_tensor(
                        out=src_tile(t), in0=src_tile(t), scalar=rstd[:, col:col + 1], in1=gB[gslot],
                        op0=ALU.mult, op1=ALU.mult), self.src_reads(t) + ["rstd%d" % col, "gb%d" % gslot],
                        self.src_reads(t), tag=self.tagp + "out")
                    dma("sp", out_d[t * 128:(t + 1) * 128, :], src_tile(t), "outs", reads=self.src_reads(t),
                        writes=["out%d" % t], tag="store")
                    return
                hb = hbf[k % 2]
                vop("dve", lambda h, t=t, col=col, hb=hb: h.scalar_tensor_tensor(
                    out=hb, in0=src_tile(t), scalar=rstd[:, col:col + 1], in1=gB[gslot],
                    op0=ALU.mult, op1=ALU.mult),
                    self.src_reads(t) + ["rstd%d" % col, "gb%d" % gslot], r_hbf(k % 2), tag=self.tagp + "h")

            def _s3(self, k):
                hb = hbf[k % 2]
                bank = self.banks[k % len(self.banks)]
                psb = ps[bank][:, :].bitcast(BF16)

                def fn(h, hb=hb, psb=psb):
                    ins = None
                    for c in range(DC):
                        ins = h.transpose(out=psb[:, c * 128:(c + 1) * 128], in_=hb[:, c * 128:(c + 1) * 128],
                                          identity=identb[:, :])
                    return ins
                sch.add("pe", fn, reads=r_hbf(k % 2) + ["identb"], writes=r_ps(bank), tag=self.tagp + "tr")

            def _s4(self, k):
                t = self.tiles[k]
                bank = self.banks[k % len(self.banks)]
                psb = ps[bank][:, :].bitcast(BF16)
                evac(self.dst[:, :, t * 128:(t + 1) * 128], psb.rearrange("p (c k) -> p c k", c=DC),
                     r_ps(bank), self.dst_regions(t), tag=self.tagp + "trev")

            def _step(self, i):
                n = len(self.tiles)
                if self.dst is not None:
                    if 0 <= i - 3 < n:
                        self._s4(i - 3)
                    if 0 <= i - 2 < n:
                        self._s3(i - 2)
                if 0 <= i - 1 < n:
                    self._s2(i - 1)
                if 0 <= i < n:
                    self._s1(i)

            def push(self, t):
                self.tiles.append(t)
                self._step(len(self.tiles) - 1)

            def flush(self):
                n = len(self.tiles)
                for i in range(n, n + 3):
                    self._step(i)

        ns1 = NormStream(lambda t: xs[:, t, :], lambda t: r_xs(t), 0, hT, lambda t: ["hT%d" % t], 0, "n1")
        def va_proj(t):
            bank = t % 2
            mm_group(ps[bank][:, :], [(hT[:, c, t * 128:(t + 1) * 128], wVa[:, c, :]) for c in range(DC)],
                     ["hT%d" % t] + r_ws(0), r_ps(bank), tag="va_mm")
            evac(Va[:, t, :, 0:64], ps[bank][:, :].rearrange("p (h k) -> p h k", h=8), r_ps(bank), r_Va(t), tag="va_ev")
        n1_order = list(range(8, 16)) + list(range(0, 8))
        for i, t in enumerate(n1_order):
            ns1.push(t)
            if i == 10:
                sch.add("pool", lambda h: h.memset(Va[:, :, :, 64:128], 1.0), reads=[], writes=["Va_ones"],
                        after=[r for t in range(NT) for r in r_Va(t)], tag="va_ones")
            if i >= 12:
                va_proj(n1_order[i - 12])
        ns1.flush()
        for i in range(4, 16):
            va_proj(n1_order[i])
        dbg_dump("hT", hT_t[:, :], ["hT%d" % t for t in range(NT)])
        dbg_dump("TT", TT_t[:, :], ["TT0", "TT1"])
        dbg_dump("rstd", rstd[:, :], ["rstd%d" % t for t in range(NT)])

        def zero_ss():
            vop("dve", lambda h: h.memset(ss[:, :], 0.0), ["ss%d" % c for c in range(18)],
                ["ss%d" % c for c in range(18)])

        wo_p = [None, None]
        dbg_dump("Va", arena[:, 16384:32768], [r for t in range(NT) for r in r_Va(t)])

        pt_ctr = {"i": 0}

        def attention_items(items, sbanks, lag, tagp="", inserts=None, pts=None, defer_steps=6):
            n = len(items)

            def do_qk(it, sb_):
                sch.add("pe", lambda h, it=it, sb_=sb_: it["qk"](h, sb_), reads=it["qk_reads"], writes=r_ps(sb_),
                        tag=tagp + "qk")
                if pts is None:
                    pb = pt_ctr["i"] % NPT
                else:
                    pb = pts[pt_ctr["i"] % len(pts)]
                pt_ctr["i"] += 1
                it["pb"] = pb
                c0, c1 = it["cols"]
                act(PT[pb][:, c0:c1], ps[sb_][:, c0:c1], AF.Exp, r_ps(sb_), r_PT(pb), tag=tagp + "exp")

            def do_pv(it):
                pb = it["pb"]
                c0, c1 = it["cols"]

                def fn(h, it=it, pb=pb, c0=c0, c1=c1):
                    ins = None
                    for (o, l, st, sp_) in it["pv"]:
                        ins = h.matmul(o[:, c0:c1], l, PT[pb][:, c0:c1], start=st, stop=sp_)
                    return ins
                wr = []
                for b_ in it["pv_banks"]:
                    wr += r_ps(b_)
                sch.add("pe", fn, reads=r_PT(pb) + it["pv_reads"], writes=wr, tag=tagp + "pv")
                if it.get("post") is not None:
                    later = it["post"]()
                    if later is not None:
                        deferred.append([defer_steps, later])

            deferred = []
            for j in range(n + lag):
                if inserts and j in inserts:
                    inserts[j]()
                for d in deferred:
                    d[0] -= 1
                for d in [d for d in deferred if d[0] <= 0]:
                    d[1]()
                    deferred.remove(d)
                if j < n:
                    do_qk(items[j], sbanks[j % len(sbanks)])
                if j - lag >= 0:
                    do_pv(items[j - lag])
            for d in deferred:
                d[1]()

        MSETS = [(0, 2), (1, 3), (4, 5)]

        def moba_set(h):
            return MSETS[h % 3]

        moba_w = {0: pre_sub}

        def moba_load_w(hp):
            subq = (hp % 2) * 2
            moba_w[hp] = (load_sub(subq, w_in_d, hp * 128, tag="wqa"),
                          load_sub(subq + 1, w_in_d, 512 + hp * 128, tag="wka"))

        def moba_proj_group(hp, which, qc):
            subq = (hp % 2) * 2
            w = moba_w[hp][which]
            bank = 4 + (qc % 2)
            mm_group(ps[bank][:, :], [(w[:, c, :], hT[:, c, qc * 512:(qc + 1) * 512]) for c in range(DC)],
                     r_hT(4 * qc, 4 * qc + 4) + ["wsub%d" % (subq + which)], r_ps(bank), tag="pa_mm")
            for hh in range(2):
                slot = moba_set(2 * hp + hh)[which]
                evac(QK[slot][0:64, qc * 512:(qc + 1) * 512], ps[bank][hh * 64:(hh + 1) * 64, :], r_ps(bank),
                     r_QK(slot, qc), scale=(0.125 if which == 0 else None), eng="dve", tag="pa_ev")

        def moba_prep_k(h):
            hh = h % 2
            qs, ks = moba_set(h)
            Bk = QK[ks]
            vop("dve", lambda hd, Bk=Bk: hd.tensor_reduce(
                out=kmf[0:64, 0:8], in_=Bk[0:64, :].rearrange("p (j l) -> p j l", j=8), axis=AX.X, op=ALU.add),
                r_QK(ks), ["kmf"], tag="kmean")
            vop("dve", lambda hd, hh=hh: hd.tensor_scalar(
                out=kmb4[0:64, hh, 0, :], in0=kmf[0:64, 0:8], scalar1=1.0 / 256.0, scalar2=None, op0=ALU.mult),
                ["kmf"], ["kmb%d" % hh], tag="kmhi")
            vop("dve", lambda hd, hh=hh: hd.scalar_tensor_tensor(
                out=kmb4[0:64, hh, 1, :], in0=kmf[0:64, 0:8], scalar=1.0 / 256.0, in1=kmb4[0:64, hh, 0, :],
                op0=ALU.mult, op1=ALU.subtract), ["kmf", "kmb%d" % hh], ["kmb%d" % hh], tag="kmlo")

        def moba_prep_gate(h):
            hh = h % 2
            qs, ks = moba_set(h)
            Aq = QK[qs]

            def gate_fn(hd, Aq=Aq, hh=hh):
                ins = None
                for qt in range(16):
                    hd.matmul(ps[7][:, qt * 8:(qt + 1) * 8], Aq[0:72, qt * 128:(qt + 1) * 128], kmb4[0:72, hh, 0, :],
                              start=True, stop=False)
                    ins = hd.matmul(ps[7][:, qt * 8:(qt + 1) * 8], Aq[0:72, qt * 128:(qt + 1) * 128],
                                    kmb4[0:72, hh, 1, :], start=False, stop=True)
                return ins
            sch.add("pe", gate_fn, reads=r_QK(qs) + ["kmb%d" % hh], writes=r_ps(7), tag="gate")
            vop("dve", lambda hd: hd.tensor_tensor(out=g2[:, :], in0=ps[7][:, 0:128], in1=negmask[:, :], op=ALU.add),
                r_ps(7) + ["negmask"], ["g2"], tag="g2")

            def top_fn(hd):
                ins = None
                for qt in range(16):
                    ins = hd.max(out=top8[:, qt * 8:(qt + 1) * 8], in_=g2[:, qt * 8:(qt + 1) * 8])
                return ins
            vop("dve", top_fn, ["g2"], ["top8"], tag="top8")
            vop("dve", lambda hd: hd.tensor_tensor(
                out=selt[:, :].rearrange("p (t j) -> p t j", t=16),
                in0=g2[:, :].rearrange("p (t j) -> p t j", t=16),
                in1=top8[:, :].rearrange("p (t j) -> p t j", t=16)[:, :, 2:3].broadcast_to([128, 16, 8]),
                op=ALU.is_ge), ["g2", "top8"], ["selt"], tag="sel")
            vop("dve", lambda hd: hd.scalar_tensor_tensor(
                out=mpad3[:, :, 64:72], in0=selt[:, :].rearrange("p (t j) -> p t j", t=16), scalar=-1.0,
                in1=ownm1[:, :].rearrange("p (t j) -> p t j", t=16), op0=ALU.add, op1=ALU.max),
                ["selt", "ownm1"], ["mpad"], tag="mval")

        def moba_prep_mask(h):
            qs, ks = moba_set(h)
            Aq = QK[qs]
            for qc in range(4):
                def mt_fn(hd, qc=qc):
                    ins = None
                    for j in range(4):
                        qt = qc * 4 + j
                        ins = hd.matmul(ps[7][0:72, j * 128:(j + 1) * 128], mpad3[:, qt, :], I3[:, :],
                                        start=True, stop=True)
                    return ins
                sch.add("pe", mt_fn, reads=["mpad", "I3"], writes=r_ps(7), tag="mtr")
                evac(Aq[64:72, qc * 512:(qc + 1) * 512], ps[7][64:72, :], r_ps(7), r_QK(qs, qc), eng="dve", tag="mtr_ev")

        def moba_items(h):
            hp, hh = h // 2, h % 2
            qs, ks = moba_set(h)
            Aq, Bk = QK[qs], QK[ks]
            items = []
            for qc in range(4):
                obank = 2 + (qc % 2)
                nkt = 4 * qc + 4
                for kt in range(nkt):
                    i_d = kt - 4 * qc
                    c0 = 128 * i_d if i_d > 0 else 0
                    adds = []
                    for j in range(4):
                        if 128 * j < c0:
                            continue
                        if kt == 4 * qc + j:
                            adds.append((j, 0))
                        elif kt == 4 * qc + j - 1:
                            adds.append((j, 1))

                    def qk_fn(hd, sbank, Aq=Aq, Bk=Bk, qc=qc, kt=kt, c0=c0, adds=adds, h=h):
                        ins = hd.matmul(ps[sbank][:, c0:512], Bk[0:72, kt * 128:(kt + 1) * 128],
                                        Aq[0:72, qc * 512 + c0:(qc + 1) * 512], start=True, stop=(len(adds) == 0))
                        for ai, (j, tsel) in enumerate(adds):
                            ins = hd.matmul(ps[sbank][:, j * 128:(j + 1) * 128], Jb[:, :], TT[:, h, tsel, :],
                                            start=False, stop=(ai == len(adds) - 1))
                        return ins
                    it = dict(qk=qk_fn, qk_reads=r_QK(qs, qc) + r_QK(ks, kt // 4) + ["TT0", "TT1", "Jb"],
                              cols=(c0, 512),
                              pv=[(ps[obank], Va[:, kt, h, :], kt == 0, kt == nkt - 1)],
                              pv_reads=r_Va(kt) + ["Va_ones"], pv_banks=[obank], post=None)
                    if kt == nkt - 1:
                        def post(hh=hh, hp=hp, qc=qc, obank=obank):
                            rc = rcb[qc % 2]
                            rrc = ["rcb%d" % (qc % 2)]
                            lo = 64 * hh
                            act(rc[lo:lo + 64, :], ps[obank][64:128, :], AF.Ln, r_ps(obank), rrc, tag="rc_ln")
                            act(rc[lo:lo + 64, :], rc[lo:lo + 64, :], AF.Exp, rrc, rrc, scale=-1.0, tag="rc")
                            vop("dve", lambda hd: hd.tensor_tensor(
                                out=mixT[lo:lo + 64, 4 * qc:4 * qc + 4, hp, :],
                                in0=ps[obank][0:64, :].rearrange("p (t k) -> p t k", t=4),
                                in1=rc[lo:lo + 64, :].rearrange("p (t k) -> p t k", t=4), op=ALU.mult),
                                r_ps(obank) + rrc, [r for t in range(4 * qc, 4 * qc + 4) for r in r_mixT(t)],
                                tag="onorm")
                        it["post"] = post
                    items.append(it)
            return items

        vop("pool", lambda h: h.memset(QK[4][64:72, :], 0.0), [], r_QK(4))
        MOBA_SB = [0, 1, 6]
        MOBA_LAG = 3
        diff_w = {}

        def diff_load_w(hb):
            subq = (hb % 2) * 2
            diff_w[hb] = (load_sub(subq, w_in_d, 1536 + hb * 128, tag="wqb"),
                          load_sub(subq + 1, w_in_d, 2048 + hb * 128, tag="wkb"))

        def diff_proj_group(hb, which, qc, bank=6):
            sl = hb % 2
            subq = (hb % 2) * 2
            w = diff_w[hb][which]
            mm_group(ps[bank][:, :], [(w[:, c, :], hT[:, c, qc * 512:(qc + 1) * 512]) for c in range(DC)],
                     r_hT(4 * qc, 4 * qc + 4) + ["wsub%d" % (subq + which)], r_ps(bank), tag="pb_mm")
            if which == 0:
                evac(QK[sl][:, qc * 512:(qc + 1) * 512], ps[bank][:, :], r_ps(bank), r_QK(sl, qc), scale=0.125,
                     eng="dve", tag="qb_ev")
            else:
                evac(QK[2 + sl][0:64, qc * 512:(qc + 1) * 512], ps[bank][0:64, :], r_ps(bank), r_QK(2 + sl, qc),
                     eng="dve", tag="kb_ev")
                evac(QK[4 + sl][64:128, qc * 512:(qc + 1) * 512], ps[bank][64:128, :], r_ps(bank), r_QK(4 + sl, qc),
                     eng="dve", tag="kb_ev")

        nsm = NormStream(lambda t: xtmp[t], lambda t: ["xtmp%d" % t], 1, mTp, lambda t: ["mTp"], 16, "nm")
        nsm.push(0)
        nsm.push(1)
        nsm.flush()
        dma("sp", gB[1], g_cross_d.partition_broadcast(128), "gb1", writes=["gb1"])

        def kc_group(ec):
            wp = kvw["p"]
            bank = 4 + ec % 2
            mm_group(ps[bank][:, 0:256], [(wp[:, c, (ec % 4) * 128:(ec % 4 + 1) * 128], mTp[:, c, :]) for c in range(8)],
                     ["mTp"] + r_ws(3), r_ps(bank), tag="kc_mm")
            evac(KcT2[:, ec, :], ps[bank][:, 0:256], r_ps(bank), ["xtmp0"], eng="dve", tag="kc_ev")

        def vc_group(mt, half):
            wp = kvw["p"]
            bank = 4 + mt % 2
            mm_group(ps[bank][:, :], [(mTp[:, c, mt * 128:(mt + 1) * 128], wp[:, c, :]) for c in range(8)],
                     ["mTp"] + r_ws(3), r_ps(bank), tag="vc_mm")
            evac(Vc2[:, mt, half * 512:(half + 1) * 512], ps[bank][:, :], r_ps(bank), ["xtmp1"], eng="dve", tag="vc_ev")

        def kv_inserts(hp):
            ins = {}
            if hp == 0:
                for i, ec in enumerate(range(0, 4)):
                    ins[17 + 4 * i] = (lambda ec=ec: kc_group(ec))
                ins[34] = (lambda: kvw.__setitem__("p", load_panel(3, w_ck_d, 512, 512, tag="wck1")))
            elif hp == 1:
                for i, ec in enumerate(range(4, 8)):
                    ins[17 + 4 * i] = (lambda ec=ec: kc_group(ec))
                ins[34] = (lambda: kvw.__setitem__("p", load_panel(3, w_cv_d, 0, 512, tag="wcv0")))
            elif hp == 2:
                ins[17] = (lambda: vc_group(0, 0))
                ins[25] = (lambda: vc_group(1, 0))
                ins[34] = (lambda: kvw.__setitem__("p", load_panel(3, w_cv_d, 512, 512, tag="wcv1")))
            else:
                ins[17] = (lambda: vc_group(0, 1))
                ins[25] = (lambda: vc_group(1, 1))
                ins[34] = (lambda: wo_p.__setitem__(0, load_panel(3, w_out_d, 0, 512, tag="wo0")))
            return ins

        for which in range(2):
            for qc in range(4):
                moba_proj_group(0, which, qc)
        moba_prep_k(0)
        moba_prep_gate(0)
        moba_prep_mask(0)
        for hp in range(4):
            hA, hB = 2 * hp, 2 * hp + 1
            itemsA = moba_items(hA)
            itemsB = moba_items(hB)
            insA = {0: (lambda hB=hB: moba_prep_k(hB)), 6: (lambda hB=hB: moba_prep_gate(hB)),
                    14: (lambda hB=hB: moba_prep_mask(hB))}
            if hp == 0:
                def _wo():
                    wo_p[1] = load_panel(0, w_out_d, 512, 512, tag="wo1")
                insA[2] = _wo
            if hp + 1 < 4:
                insA[4] = (lambda hp=hp: moba_load_w(hp + 1))
            else:
                insA[4] = (lambda: diff_load_w(0))
            for k_, v_ in kv_inserts(hp).items():
                assert k_ not in insA
                insA[k_] = v_
            attention_items(itemsA, MOBA_SB, MOBA_LAG, tagp="moba", inserts=insA)
            insB = {}
            if hp == 3:
                def _diff_pre():
                    vop("pool", lambda h: h.memset(QK[2][64:128, :], 0.0), [], r_QK(2))
                    vop("pool", lambda h: h.memset(QK[4][0:64, :], 0.0), [], r_QK(4))
                insB[1] = _diff_pre
                k = 0
                for which in range(2):
                    for qc in range(4):
                        insB[5 + 4 * k] = (lambda which=which, qc=qc: diff_proj_group(0, which, qc, bank=4 + (qc % 2)))
                        k += 1
            if hp + 1 < 4:
                k = 0
                for which in (1, 0):
                    for qc in range(4):
                        insB[3 + 4 * k] = (lambda hp=hp, which=which, qc=qc: moba_proj_group(hp + 1, which, qc))
                        k += 1
                insB[20] = (lambda hp=hp: moba_prep_k(2 * hp + 2))
                insB[36] = (lambda hp=hp: moba_prep_gate(2 * hp + 2))
                insB[42] = (lambda hp=hp: moba_prep_mask(2 * hp + 2))
            attention_items(itemsB, MOBA_SB, MOBA_LAG, tagp="moba", inserts=insB)

        dbg_dump("mixT_raw", arena[:, 0:16384], [r for t in range(NT) for r in r_mixT(t)])
        for qc in range(4):
            mreg = [r for t in range(4 * qc, 4 * qc + 4) for r in r_mixT(t)]
            for c in range(4):
                act(osq.rearrange("p (t k) -> p t k", t=4), mixT[:, 4 * qc:4 * qc + 4, c, :], AF.Square, mreg, R_OSQ,
                    tag="msq")
                mm_group(ps[7][:, :], [(ones_b[:, :], osq)], R_OSQ + ["ones_b"], r_ps(7),
                         start_first=(c == 0), stop_last=(c == 3), tag="msum")
            rc = rcb[qc % 2]
            rrc = ["rcb%d" % (qc % 2)]
            act(rc, ps[7][:, :], AF.Ln, r_ps(7) + ["epsb"], rrc, scale=1.0 / 512.0, bias=epsb[:, 0:1], tag="mln")
            act(rc, rc, AF.Exp, rrc, rrc, scale=-0.5, tag="mrs")
            for c in range(4):
                vop("dve", lambda hd, rc=rc, c=c, qc=qc: hd.scalar_tensor_tensor(
                    out=mixT[:, 4 * qc:4 * qc + 4, c, :], in0=mixT[:, 4 * qc:4 * qc + 4, c, :], scalar=gA[:, c:c + 1],
                    in1=rc.rearrange("p (t k) -> p t k", t=4), op0=ALU.mult, op1=ALU.mult),
                    mreg + rrc + ["gA"], mreg, tag="mscale")

        for t in range(NT):
            bank = 4 + (t % 2)
            mm_group(ps[bank][:, :], [(hT[:, c, t * 128:(t + 1) * 128], wVb[:, c, :]) for c in range(DC)],
                     ["hT%d" % t] + r_ws(1), r_ps(bank), tag="vb_mm")
            evac(Vb[:, t, :, :], ps[bank][:, :].rearrange("p (h k) -> p h k", h=4), r_ps(bank), r_Vb(t), tag="vb_ev")

        vop("pool", lambda h: h.memset(QK[3][64:128, :], 0.0), [], r_QK(3))
        vop("pool", lambda h: h.memset(QK[5][0:64, :], 0.0), [], r_QK(5))

        def diff_items(hb):
            h = 8 + hb
            sl = hb % 2
            Aq = QK[sl]
            items = []
            for qc in range(4):
                nkt = 4 * qc + 4
                for kt in range(nkt):
                    i_d = kt - 4 * qc
                    c0 = 128 * i_d if i_d > 0 else 0
                    adds = []
                    for j in range(4):
                        if 128 * j < c0:
                            continue
                        if kt == 4 * qc + j:
                            adds.append((j, 0))
                        elif kt == 4 * qc + j - 1:
                            adds.append((j, 1))
                    for m in range(2):
                        Km = QK[2 + 2 * m + sl]

                        def qk_fn(hd, sbank, Aq=Aq, Km=Km, qc=qc, kt=kt, c0=c0, adds=adds, h=h):
                            ins = hd.matmul(ps[sbank][:, c0:512], Km[:, kt * 128:(kt + 1) * 128],
                                            Aq[:, qc * 512 + c0:(qc + 1) * 512], start=True, stop=(len(adds) == 0))
                            for ai, (j, tsel) in enumerate(adds):
                                ins = hd.matmul(ps[sbank][:, j * 128:(j + 1) * 128], Jb[:, :], TT[:, h, tsel, :],
                                                start=False, stop=(ai == len(adds) - 1))
                            return ins
                        it = dict(qk=qk_fn, qk_reads=r_QK(sl, qc) + r_QK(2 + 2 * m + sl, kt // 4) + ["TT0", "TT1", "Jb"],
                                  cols=(c0, 512),
                                  pv=[(ps[2 + m], Vb[:, kt, hb, :], kt == 0, kt == nkt - 1),
                                      (ps[4 + m], ones_b[:, :], kt == 0, kt == nkt - 1)],
                                  pv_reads=r_Vb(kt) + ["ones_b"], pv_banks=[2 + m, 4 + m], post=None)
                        if kt == nkt - 1 and m == 1:
                            def post(hb=hb, qc=qc):
                                r0, r1, s0, s1 = rcb[0], rcb[1], rcb[2], rcb[3]
                                mreg = [r for t in range(4 * qc, 4 * qc + 4) for r in r_mixT(t)]
                                vop("dve", lambda hd: hd.tensor_copy(out=r0, in_=ps[2][:, :]), r_ps(2), ["rcb0"], tag="d_c0")
                                vop("dve", lambda hd: hd.tensor_copy(out=r1, in_=ps[3][:, :]), r_ps(3), ["rcb1"], tag="d_c1")
                                act(s0, ps[4][:, :], AF.Ln, r_ps(4), ["rcb2"], tag="d_ln0")
                                act(s1, ps[5][:, :], AF.Ln, r_ps(5), ["rcb3"], tag="d_ln1")
                                act(s0, s0, AF.Exp, ["rcb2"], ["rcb2"], scale=-1.0, tag="d_r0")
                                act(s1, s1, AF.Exp, ["rcb3"], ["rcb3"], scale=-1.0, tag="d_r1")
                                vop("dve", lambda hd: hd.tensor_tensor(out=r0, in0=r0, in1=s0, op=ALU.mult),
                                    ["rcb0", "rcb2"], ["rcb0"], tag="d_a0")
                                vop("dve", lambda hd: hd.tensor_tensor(out=r1, in0=r1, in1=s1, op=ALU.mult),
                                    ["rcb1", "rcb3"], ["rcb1"], tag="d_a1")
                                vop("dve", lambda hd: hd.scalar_tensor_tensor(
                                    out=r0, in0=r1, scalar=neglam, in1=r0, op0=ALU.mult, op1=ALU.add),
                                    ["rcb0", "rcb1", "lams"], ["rcb0"], tag="d_o")
                                vop("dve", lambda hd: hd.tensor_tensor(out=osq, in0=r0, in1=r0, op=ALU.mult),
                                    ["rcb0"], R_OSQ, tag="d_sq")

                                def post_b(hb=hb, qc=qc, r0=r0, s0=s0, mreg=mreg):
                                    mm_group(ps[6][:, :], [(ones_b[:, :], osq)], R_OSQ + ["ones_b"], r_ps(6), tag="d_ss")
                                    act(s0, ps[6][:, :], AF.Ln, r_ps(6) + ["epsb"], ["rcb2"], scale=1.0 / 128.0,
                                        bias=epsb[:, 0:1], tag="d_lnss")
                                    act(s0, s0, AF.Exp, ["rcb2"], ["rcb2"], scale=-0.5, tag="d_rs")
                                    vop("dve", lambda hd: hd.scalar_tensor_tensor(
                                        out=mixT[:, 4 * qc:4 * qc + 4, 4 + hb, :],
                                        in0=r0.rearrange("p (t k) -> p t k", t=4), scalar=gS8[:, 0:1],
                                        in1=s0.rearrange("p (t k) -> p t k", t=4), op0=ALU.mult, op1=ALU.mult),
                                        ["rcb0", "rcb2", "gS8"], mreg, tag="d_out")
                                return post_b
                            it["post"] = post
                        items.append(it)
            return items

        DIFF_SB = [0, 1, 7]
        DIFF_LAG = 3
        for hb in range(4):
            items = diff_items(hb)
            ins = {}
            if hb + 1 < 4:
                ins[2] = (lambda hb=hb: diff_load_w(hb + 1))
                k = 0
                for which in range(2):
                    for qc in range(4):
                        ins[12 + 8 * k] = (lambda hb=hb, which=which, qc=qc: diff_proj_group(hb + 1, which, qc))
                        k += 1
            attention_items(items, DIFF_SB, DIFF_LAG, tagp="diff", inserts=ins)

        dbg_dump("mixT", arena[:, 0:16384], [r for t in range(NT) for r in r_mixT(t)])
        dma("sp", gB[0], g_ffn_d.partition_broadcast(128), "gb0", writes=["gb0"])
        zero_ss()
        ns2 = NormStream(lambda t: xs[:, t, :], lambda t: r_xs(t), 1, hT, lambda t: ["hT%d" % t], 0, "n2")
        order = list(range(8, 16)) + list(range(7, -1, -1))
        for i, t in enumerate(order):
            xb = i % 2
            r_xre = r_a2(xb * 2048, (xb + 1) * 2048)
            dma("sp", xre[xb], x_d[t * 128:(t + 1) * 128, :], "xre%d" % xb, writes=r_xre, tag="xre")
            for half in range(2):
                bank = (2 * i + half) % 4
                mm_group(ps[bank][:, :], [(mixT[:, t, c, :], wo_p[half][:, c, :]) for c in range(8)],
                         r_mixT(t) + r_ws(3 if half == 0 else 0), r_ps(bank), tag="wo_mm")
            for half in range(2):
                bank = (2 * i + half) % 4
                vop("dve", lambda hd, t=t, half=half, bank=bank, xb=xb: hd.tensor_tensor(
                    out=xs[:, t, half * 512:(half + 1) * 512], in0=ps[bank][:, :],
                    in1=xre[xb][:, half * 512:(half + 1) * 512], op=ALU.add),
                    r_ps(bank) + r_xre, r_xs(t), tag="wo_ev")
            if STOP_AFTER != "mix":
                ns2.push(t)
        if STOP_AFTER != "mix":
            ns2.flush()

        def final_out_only():
            pass

        def phase_cross():
            QcA = a2[:, 8192:12288].rearrange("p (c k) -> p c k", c=8)

            def r_qca(g):
                return r_a2(8192 + g * 512, 8192 + (g + 1) * 512)
            dma("sp", gB[1], g_fin_d.partition_broadcast(128), "gb1", writes=["gb1"])
            wcq = [load_panel(1, w_cq_d, 0, 512, tag="wcq0"), load_panel(2, w_cq_d, 512, 512, tag="wcq1")]
            wco = [load_panel(3, w_co_d, 0, 512, tag="wco0"), load_panel(0, w_co_d, 512, 512, tag="wco1")]
            rwcq = [r_ws(1), r_ws(2)]
            rwco = [r_ws(3), r_ws(0)]
            zero_ss()
            ns3 = NormStream(lambda t: xs[:, t, :], lambda t: r_xs(t), 0, hT, lambda t: ["hT%d" % t], 0, "n3", banks=(7,))

            def qproj(qc, g):
                hd_, e2 = g // 2, g % 2
                bank = 5 + (g % 2)
                col = ((hd_ % 2) * 2 + e2) * 128
                mm_group(ps[bank][:, :], [(wcq[hd_ // 2][:, c, col:col + 128], hT[:, c, qc * 512:(qc + 1) * 512])
                                          for c in range(8)],
                         r_hT(4 * qc, 4 * qc + 4) + rwcq[hd_ // 2], r_ps(bank), tag="qc_mm")
                evac(QcA[:, g, :], ps[bank][:, :], r_ps(bank), r_qca(g), scale=1.0 / 16.0, tag="qc_ev")

            for g in range(8):
                qproj(0, g)
            for qc in range(4):
                ob = qc % 2
                r_oc = r_a2(ob * 4096, (ob + 1) * 4096)
                items = []
                for hd_ in range(4):
                    for mt in range(2):
                        def qk_fn(hd, sbank, hd_=hd_, mt=mt):
                            ins = None
                            for e2 in range(2):
                                ins = hd.matmul(ps[sbank][:, :], KcT2[:, 2 * hd_ + e2, mt * 128:(mt + 1) * 128],
                                                QcA[:, 2 * hd_ + e2, :], start=(e2 == 0), stop=(e2 == 1))
                            return ins
                        it = dict(qk=qk_fn, qk_reads=r_qca(2 * hd_) + r_qca(2 * hd_ + 1) + ["xtmp0"], cols=(0, 512),
                                  pv=[(ps[2 + dv2], Vc2[:, mt, (2 * hd_ + dv2) * 128:(2 * hd_ + dv2 + 1) * 128], mt == 0, mt == 1)
                                      for dv2 in range(2)] + [(ps[4], ones_b[:, :], mt == 0, mt == 1)],
                                  pv_reads=["xtmp1", "ones_b"], pv_banks=[2, 3, 4], post=None)
                        if mt == 1:
                            def post(hd_=hd_, ob=ob, r_oc=r_oc):
                                c0, c1, rc = rcb[0], rcb[1], rcb[3]
                                vop("dve", lambda hd: hd.tensor_copy(out=c0, in_=ps[2][:, :]), r_ps(2), ["rcb0"], tag="c_c0")
                                vop("dve", lambda hd: hd.tensor_copy(out=c1, in_=ps[3][:, :]), r_ps(3), ["rcb1"], tag="c_c1")
                                act(rc, ps[4][:, :], AF.Ln, r_ps(4), ["rcb3"], tag="c_ln")
                                act(rc, rc, AF.Exp, ["rcb3"], ["rcb3"], scale=-1.0, tag="c_rc")
                                vop("dve", lambda hd: hd.tensor_tensor(out=ocT[ob][:, 2 * hd_, :], in0=c0, in1=rc, op=ALU.mult),
                                    ["rcb0", "rcb3"], r_oc, tag="c_on")
                                vop("dve", lambda hd: hd.tensor_tensor(out=ocT[ob][:, 2 * hd_ + 1, :], in0=c1, in1=rc, op=ALU.mult),
                                    ["rcb1", "rcb3"], r_oc, tag="c_on")
                            it["post"] = post
                        items.append(it)
                ins = {}
                if qc + 1 < 4:
                    for hd_ in range(4):
                        ins[2 * hd_ + 2] = (lambda qc=qc, hd_=hd_: (qproj(qc + 1, 2 * hd_), qproj(qc + 1, 2 * hd_ + 1)))
                attention_items(items, [0, 1], 2, tagp="cross", inserts=ins, pts=[4, 5, 6])
                for tt in range(4):
                    t = 4 * qc + tt
                    for half in range(2):
                        bank = 5 + half
                        mm_group(ps[bank][:, :], [(ocT[ob][:, c, tt * 128:(tt + 1) * 128], wco[half][:, c, :]) for c in range(8)],
                                 r_oc + rwco[half], r_ps(bank), tag="co_mm")
                        vop("dve", lambda hd, t=t, half=half, bank=bank: hd.tensor_tensor(
                            out=xs[:, t, half * 512:(half + 1) * 512], in0=ps[bank][:, :],
                            in1=xs[:, t, half * 512:(half + 1) * 512], op=ALU.add),
                            r_ps(bank) + r_xs(t), r_xs(t), tag="co_ev")
                    if STOP_AFTER != "cross":
                        ns3.push(t)
            if STOP_AFTER != "cross":
                ns3.flush()

        def phase_ffn():
            groups = [(2560, 2)] + [(g * 512, 4) for g in range(5)]
            panel_i = {"i": 1}

            def next_slot():
                s = panel_i["i"] % 4
                panel_i["i"] += 1
                return s
            loaded = []

            def load_group(gi):
                f0, nch = groups[gi]
                sg = next_slot()
                wg = load_panel(sg, w_gate_d, f0, nch * 128, tag="wg")
                su = next_slot()
                wu = load_panel(su, w_up_d, f0, nch * 128, tag="wu")
                sd_ = next_slot()
                src = w_down_d[f0:f0 + nch * 128, :].rearrange("(c p) n -> p c n", p=128)
                dst = wsl[sd_][:, 0:nch * 1024].rearrange("p (c n) -> p c n", c=nch)
                dma("pool", dst, src, "ws%d" % sd_, writes=r_ws(sd_), tag="wd")
                loaded.append((wg, r_ws(sg), wu, r_ws(su), dst, r_ws(sd_)))
            load_group(0)
            zero_ss()
            nsf = NormStream(lambda t: xs[:, t, :], lambda t: r_xs(t), 1, None, None, 0, "nf")
            for gi, (f0, nch) in enumerate(groups):
                wg, rwg, wu, rwu, wd, rwd = loaded[gi]
                ab = gi % 2
                r_act = r_a2(ab * 8192, (ab + 1) * 8192)
                for fc in range(nch):
                    for qc in range(4):
                        bg = (2 * qc) % 4
                        bu = bg + 1
                        mm_group(ps[bg][:, :], [(wg[:, c, fc * 128:(fc + 1) * 128], hT[:, c, qc * 512:(qc + 1) * 512])
                                                for c in range(8)], r_hT(4 * qc, 4 * qc + 4) + rwg, r_ps(bg), tag="g_mm")
                        mm_group(ps[bu][:, :], [(wu[:, c, fc * 128:(fc + 1) * 128], hT[:, c, qc * 512:(qc + 1) * 512])
                                                for c in range(8)], r_hT(4 * qc, 4 * qc + 4) + rwu, r_ps(bu), tag="u_mm")
                        sgb = rcb[qc % 2]
                        rsg = ["rcb%d" % (qc % 2)]
                        act(sgb, ps[bg][:, :], AF.Silu, r_ps(bg), rsg, tag="silu")
                        vop("dve", lambda hd, ab=ab, fc=fc, qc=qc, bu=bu, sgb=sgb: hd.tensor_tensor(
                            out=actT[ab][:, fc, qc * 512:(qc + 1) * 512], in0=ps[bu][:, :], in1=sgb, op=ALU.mult),
                            r_ps(bu) + rsg, r_act, tag="gu")
                if gi + 1 < len(groups):
                    load_group(gi + 1)
                for t in range(NT):
                    for half in range(2):
                        bank = 4 + (2 * t + half) % 4
                        mm_group(ps[bank][:, :], [(actT[ab][:, fc, t * 128:(t + 1) * 128], wd[:, fc, half * 512:(half + 1) * 512])
                                                  for fc in range(nch)], r_act + rwd, r_ps(bank), tag="d_mm")
                        vop("dve", lambda hd, t=t, half=half, bank=bank: hd.tensor_tensor(
                            out=xs[:, t, half * 512:(half + 1) * 512], in0=ps[bank][:, :],
                            in1=xs[:, t, half * 512:(half + 1) * 512], op=ALU.add),
                            r_ps(bank) + r_xs(t), r_xs(t), tag="d_ev")
                    if gi == len(groups) - 1:
                        nsf.push(t)
            nsf.flush()
            sch.add("sp", lambda h: None, reads=["out%d" % t for t in range(NT)] + ["dbgout_" + n for n in dbg_d],
                    writes=[], tag="final")

        def phase_final(do_norm=True):
            if do_norm:
                dma("sp", gB[1], g_fin_d.partition_broadcast(128), "gb1", writes=["gb1"])
                zero_ss()
            outs = []
            for t in range(NT):
                if do_norm:
                    col = t
                    act(junk, xs[:, t, :], AF.Square, r_xs(t), R_JUNK + ["ss%d" % col], scale=1.0 / 32.0,
                        accum_out=ss[:, col:col + 1], tag="fsq")
                    act(sd[:, col:col + 1], ss[:, col:col + 1], AF.Sqrt, ["ss%d" % col, "epsb"], ["sd%d" % col],
                        bias=epsb[:, 0:1], tag="fsqrt")
                    vop("dve", lambda h, col=col: h.reciprocal(out=rstd[:, col:col + 1], in_=sd[:, col:col + 1]),
                        ["sd%d" % col], ["rstd%d" % col])
                    vop("dve", lambda h, t=t, col=col: h.scalar_tensor_tensor(
                        out=xs[:, t, :], in0=xs[:, t, :], scalar=rstd[:, col:col + 1], in1=gB[1],
                        op0=ALU.mult, op1=ALU.mult), r_xs(t) + ["rstd%d" % col, "gb1"], r_xs(t), tag="fout")
                outs.append(dma("sp", out_d[t * 128:(t + 1) * 128, :], xs[:, t, :], "outs", reads=r_xs(t),
                                writes=["out%d" % t], tag="store"))
            op = sch.add("sp", lambda h: None, reads=["out%d" % t for t in range(NT)] +
                         ["dbgout_" + n for n in dbg_d], writes=[], tag="final")

        if STOP_AFTER == "mix":
            phase_final(do_norm=False)
        elif STOP_AFTER == "cross":
            phase_cross()
            phase_final(do_norm=False)
        else:
            phase_cross()
            phase_ffn()

        sch.finalize()

        @block.sync
        def _(h):
            sch.emit("sp", h, semh)

        @block.tensor
        def _(h):
            sch.emit("pe", h, semh)

        @block.scalar
        def _(h):
            sch.emit("act", h, semh)

        @block.vector
        def _(h):
            sch.emit("dve", h, semh)

        @block.gpsimd
        def _(h):
            sch.emit("pool", h, semh)

    return nc


_CACHE = {}


def kernel(**inputs):
    consts = _host_consts()
    f32 = lambda a: np.ascontiguousarray(np.asarray(a, dtype=np.float32))
    shared = {
        "mix_norm_g": f32(inputs["mix_norm_g"]).reshape(D),
        "w_in": f32(inputs["w_in"]).reshape(D, 3072),
        "moba_out_g": np.ascontiguousarray(f32(inputs["moba_out_g"]).reshape(4, 128).T),
        "diff_lambda": f32(inputs["diff_lambda"]).reshape(256),
        "diff_subln_g": f32(inputs["diff_subln_g"]).reshape(128, 1),
        "w_out": f32(inputs["w_out"]).reshape(D, D),
        "rel_bias_table": f32(inputs["rel_bias_table"]).reshape(32, 12),
        "cross_norm_g": f32(inputs["cross_norm_g"]).reshape(D),
        "mem_norm_g": f32(inputs["mem_norm_g"]).reshape(D),
        "w_cq": f32(inputs["w_cq"]).reshape(D, D),
        "w_ck": f32(inputs["w_ck"]).reshape(D, D),
        "w_cv": f32(inputs["w_cv"]).reshape(D, D),
        "w_co": f32(inputs["w_co"]).reshape(D, D),
        "ffn_norm_g": f32(inputs["ffn_norm_g"]).reshape(D),
        "w_gate": f32(inputs["w_gate"]).reshape(D, DFF),
        "w_up": f32(inputs["w_up"]).reshape(D, DFF),
        "w_down": f32(inputs["w_down"]).reshape(DFF, D),
        "final_norm_g": f32(inputs["final_norm_g"]).reshape(D),
    }
    shared.update(consts)
    x = f32(inputs["x"])
    mem = f32(inputs["mem"])
    nb = x.shape[0]
    in_maps = []
    for b in range(nb):
        m = dict(shared)
        m["x"] = np.ascontiguousarray(x[b])
        m["mem"] = np.ascontiguousarray(mem[b])
        in_maps.append(m)
    nc = build_program()
    res = run_bass_kernel_spmd(nc, in_maps, core_ids=list(range(nb)))
    out = np.stack([np.asarray(r["out"], dtype=np.float32) for r in res.results], axis=0)
    if DEBUG_OUT:
        kernel.debug = [{k: np.asarray(v) for k, v in r.items() if k.startswith("dbg_")} for r in res.results]
    return out
```
